# Optimizing a Trainium2 kernel written in Bass

```python
import math
import jax, jax.numpy as jnp
from jax import lax
import numpy as np

D_MODEL = 1024
BATCH = 4
SEQ = 4096
DEPTH = 2
DEC_BATCH = 32
DEC_SEQ = 1
PAST_LEN = 8192
PAGE_SIZE = 128

N_EVEN = (DEPTH + 1) // 2
N_ODD = DEPTH // 2
D_CONV = D_MODEL // 2
CONV_W = 3
H_B = 4
DH_B = 64
ROT_DIM = DH_B // 4
ROPE_THETA = 500000.0
Q_BLOCK = 128
H_C = 4
DK_C = (D_MODEL // 2) // H_C
DV_C = D_MODEL // H_C
CHUNK = 128
N_GROUPS = 4
EXP_PER_GROUP = 4
N_EXPERTS = N_GROUPS * EXP_PER_GROUP
TOP_K = 2
D_EXPERT = 512
D_PLE = 256
LN_EPS = 1e-5
DEEPNORM_ALPHA = (2 * DEPTH) ** 0.25
DEEPNORM_BETA = (8 * DEPTH) ** -0.25

QK_B = 2 * H_B * DH_B
V_B = H_B * 2 * DH_B
EVEN_WIDTHS = [D_CONV, D_CONV, D_CONV, QK_B, QK_B, V_B]
EVEN_SPLITS = [int(s) for s in np.cumsum(EVEN_WIDTHS)[:-1]]
D_IN_EVEN = sum(EVEN_WIDTHS)
D_MIX_EVEN = D_CONV + V_B
ODD_WIDTHS = [H_C * DK_C, H_C * DK_C, H_C * DV_C, H_C * DV_C, 2 * H_C]
ODD_SPLITS = [int(s) for s in np.cumsum(ODD_WIDTHS)[:-1]]
D_IN_ODD = sum(ODD_WIDTHS)
D_MIX_ODD = H_C * DV_C

kernel_name = 'hybrid_conv_diffattn_mlstm_hmoe_step'


def layer_norm(x, g, b):
    xf = x.astype(jnp.float32)
    mu = jnp.mean(xf, axis=-1, keepdims=True)
    var = jnp.mean(jnp.square(xf - mu), axis=-1, keepdims=True)
    return ((xf - mu) * lax.rsqrt(var + LN_EPS)).astype(x.dtype) * g + b


def rms_norm(x, w):
    xf = x.astype(jnp.float32)
    return (xf * lax.rsqrt(jnp.mean(jnp.square(xf), axis=-1, keepdims=True) + LN_EPS)).astype(x.dtype) * w


def partial_rope(x, pos):
    half = ROT_DIM // 2
    inv = ROPE_THETA ** (-jnp.arange(half, dtype=jnp.float32) / half)
    ang = pos.astype(jnp.float32)[:, None] * inv[None, :]
    cos = jnp.cos(ang)[:, None, :].astype(x.dtype)
    sin = jnp.sin(ang)[:, None, :].astype(x.dtype)
    x1, x2 = x[..., :half], x[..., half:ROT_DIM]
    return jnp.concatenate([x1 * cos - x2 * sin, x2 * cos + x1 * sin, x[..., ROT_DIM:]], axis=-1)


def diff_attn_block(q, k, v, q_pos, k_pos, lam):
    b, tq = q.shape[:2]
    tk = k.shape[1]
    s = jnp.einsum('bqhd,bkhd->bhqk', q, k).astype(jnp.float32) * (DH_B ** -0.5)
    s = jnp.where(k_pos[None, :] <= q_pos[:, None], s, -jnp.inf)
    a = jax.nn.softmax(s, axis=-1).reshape(b, H_B, 2, tq, tk)
    a = (a[:, :, 0] - lam * a[:, :, 1]).astype(v.dtype)
    return jnp.einsum('bhqk,bkhe->bqhe', a, v)


def diff_attention_prompt(q, k, v, pos, lam):
    b, t = q.shape[:2]
    nb = t // Q_BLOCK
    qb = jnp.moveaxis(q.reshape(b, nb, Q_BLOCK, 2 * H_B, DH_B), 1, 0)
    pb = pos.reshape(nb, Q_BLOCK)
    ob = lax.map(lambda qp: diff_attn_block(qp[0], k, v, qp[1], pos, lam), (qb, pb))
    return jnp.moveaxis(ob, 0, 1).reshape(b, t, H_B, 2 * DH_B)


def even_mixer(x, conv_prev, pos, attend, lam_init, w_in, conv_w, subln_w, w_out):
    b, t, _ = x.shape
    gb, gc, xin, q, k, v = jnp.split(x @ w_in, EVEN_SPLITS, axis=-1)
    u = gc * xin
    full = jnp.concatenate([conv_prev.astype(u.dtype), u], axis=1)
    conv = sum(full[:, j:j + t] * conv_w[j] for j in range(CONV_W))
    y_conv = gb * conv
    q = partial_rope(q.reshape(b, t, 2 * H_B, DH_B), pos)
    k = partial_rope(k.reshape(b, t, 2 * H_B, DH_B), pos)
    v = v.reshape(b, t, H_B, 2 * DH_B)
    o = rms_norm(attend(q, k, v), subln_w) * (1.0 - lam_init)
    y = jnp.concatenate([y_conv, o.reshape(b, t, V_B)], axis=-1) @ w_out
    return y, k, v, full[:, t:]


def mlstm_chunk(q, k, v, ig, logf, C0, n0, m0):
    L = q.shape[2]
    bcum = jnp.cumsum(logf, axis=-1)
    causal = jnp.tril(jnp.ones((L, L), dtype=bool))
    dmat = jnp.where(causal, bcum[..., :, None] - bcum[..., None, :] + ig[..., None, :], -jnp.inf)
    inter = bcum + m0[..., None]
    m = jnp.maximum(inter, jnp.max(dmat, axis=-1))
    w = jnp.exp(dmat - m[..., None])
    g = jnp.exp(inter - m)
    s = jnp.einsum('bhtd,bhsd->bhts', q, k) * w
    num = g[..., None] * jnp.einsum('bhtd,bhde->bhte', q, C0) + jnp.einsum('bhts,bhse->bhte', s, v)
    den = g * jnp.einsum('bhtd,bhd->bht', q, n0) + jnp.sum(s, axis=-1)
    h = num / jnp.maximum(jnp.abs(den), jnp.exp(-m))[..., None]
    m_last = m[..., -1]
    w_last = jnp.exp(bcum[..., -1:] - bcum + ig - m_last[..., None])
    g_last = jnp.exp(bcum[..., -1] + m0 - m_last)
    C = g_last[..., None, None] * C0 + jnp.einsum('bhs,bhsd,bhse->bhde', w_last, k, v)
    n = g_last[..., None] * n0 + jnp.einsum('bhs,bhsd->bhd', w_last, k)
    return h, (C, n, m_last)


def mlstm_sequence(q, k, v, ig, logf, C0, n0, m0):
    b, h, t, _ = q.shape
    L = CHUNK if t % CHUNK == 0 else t
    nc = t // L
    to_chunks = lambda a: jnp.moveaxis(a.reshape(b, h, nc, L, *a.shape[3:]), 2, 0)
    xs = (to_chunks(q), to_chunks(k), to_chunks(v), to_chunks(ig), to_chunks(logf))

    def step(carry, inp):
        hc, carry = mlstm_chunk(*inp, *carry)
        return carry, hc

    (C, n, m), hs = lax.scan(step, (C0, n0, m0), xs)
    return jnp.moveaxis(hs, 0, 2).reshape(b, h, t, DV_C), C, n, m


def odd_mixer(x, C0, n0, m0, w_in, b_gates, norm_w, w_out):
    b, t, _ = x.shape
    q, k, v, o, g = jnp.split(x @ w_in, ODD_SPLITS, axis=-1)
    heads = lambda a, d: jnp.moveaxis(a.reshape(b, t, H_C, d), 2, 1).astype(jnp.float32)
    q = heads(q, DK_C) * (DK_C ** -0.5)
    k = heads(k, DK_C)
    v = heads(v, DV_C)
    g = jnp.moveaxis((g + b_gates).astype(jnp.float32), -1, 1)
    ig = g[:, :H_C]
    logf = jax.nn.log_sigmoid(g[:, H_C:])
    hseq, C, n, m = mlstm_sequence(q, k, v, ig, logf, C0.astype(jnp.float32),
                                   n0.astype(jnp.float32), m0.astype(jnp.float32))
    hseq = jnp.moveaxis(hseq, 1, 2)
    mu = jnp.mean(hseq, axis=-1, keepdims=True)
    var = jnp.mean(jnp.square(hseq - mu), axis=-1, keepdims=True)
    hn = ((hseq - mu) * lax.rsqrt(var + LN_EPS)).reshape(b, t, D_MIX_ODD).astype(x.dtype) * norm_w
    return (jax.nn.sigmoid(o) * hn) @ w_out, C, n, m


def hier_moe(x, w_group, w_router, w_gate, w_up, w_down):
    b, t, d = x.shape
    xt = x.reshape(b * t, d)
    gl = (xt @ w_group).astype(jnp.float32)
    gp = jax.nn.softmax(gl, axis=-1)
    g_w, g_idx = lax.top_k(gp, 1)
    el = (xt @ w_router).astype(jnp.float32).reshape(b * t, N_GROUPS, EXP_PER_GROUP)
    el = jnp.take_along_axis(el, g_idx[:, :, None], axis=1)[:, 0]
    top_w, top_i = lax.top_k(jax.nn.softmax(el, axis=-1), TOP_K)
    top_w = top_w / jnp.sum(top_w, axis=-1, keepdims=True) * g_w
    e_idx = g_idx * EXP_PER_GROUP + top_i
    gate = jnp.sum(jax.nn.one_hot(e_idx, N_EXPERTS, dtype=jnp.float32) * top_w[..., None], axis=1).astype(x.dtype)
    y = jnp.zeros_like(xt)
    for e in range(N_EXPERTS):
        hid = jax.nn.silu(xt @ w_gate[e]) * (xt @ w_up[e])
        y = y + gate[:, e:e + 1] * (hid @ w_down[e])
    return y.reshape(b, t, d)


def layer_tail(x, mix, p, ln1_g, ln1_b, ln2_g, ln2_b, wg, wr, we_g, we_u, we_d, wpp, wpg):
    x = layer_norm(DEEPNORM_ALPHA * x + mix, ln1_g, ln1_b)
    x = layer_norm(DEEPNORM_ALPHA * x + hier_moe(x, wg, wr, we_g, we_u, we_d), ln2_g, ln2_b)
    return x + jax.nn.sigmoid(x @ wpg) * (p @ wpp)


def setup_inputs(seed: int = 0) -> dict:
    key = jax.random.key(seed)
    keys = jax.random.split(key, 48)
    ctr = iter(range(48))
    nrm = lambda shape, scale: jax.random.normal(keys[next(ctr)], shape, jnp.float32) * scale
    n_pages = PAST_LEN // PAGE_SIZE
    n_used = DEC_BATCH * n_pages
    n_pool = n_used + (n_used + 3) // 4
    page_table = jax.random.permutation(keys[next(ctr)], n_pool)[:n_used].reshape(DEC_BATCH, n_pages).astype(jnp.int32)
    beta = DEEPNORM_BETA
    f_bias = jnp.broadcast_to(jnp.linspace(3.0, 6.0, H_C, dtype=jnp.float32), (N_ODD, H_C)) + nrm((N_ODD, H_C), 0.1)
    b_gates_odd = jnp.concatenate([nrm((N_ODD, H_C), 0.1), f_bias], axis=-1)
    return {
        'x_prompt': nrm((BATCH, SEQ, D_MODEL), 1.0),
        'x_sample': nrm((DEC_BATCH, DEC_SEQ, D_MODEL), 1.0),
        'cache_k': nrm((N_EVEN, n_pool, PAGE_SIZE, 2 * H_B, DH_B), 1.0),
        'cache_v': nrm((N_EVEN, n_pool, PAGE_SIZE, H_B, 2 * DH_B), 1.0),
        'page_table': page_table,
        'state_conv': nrm((N_EVEN, DEC_BATCH, CONV_W - 1, D_CONV), 1.0),
        'state_mlstm_C': nrm((N_ODD, DEC_BATCH, H_C, DK_C, DV_C), 0.1),
        'state_mlstm_n': nrm((N_ODD, DEC_BATCH, H_C, DK_C), 0.1),
        'state_mlstm_m': nrm((N_ODD, DEC_BATCH, H_C), 1.0),
        'p_prompt': nrm((DEPTH, BATCH, SEQ, D_PLE), 1.0),
        'p_sample': nrm((DEPTH, DEC_BATCH, DEC_SEQ, D_PLE), 1.0),
        'w_in_even': nrm((N_EVEN, D_MODEL, D_IN_EVEN), D_MODEL ** -0.5),
        'conv_w': nrm((N_EVEN, CONV_W, D_CONV), CONV_W ** -0.5),
        'lambda_q1': nrm((N_EVEN, DH_B), 0.1),
        'lambda_k1': nrm((N_EVEN, DH_B), 0.1),
        'lambda_q2': nrm((N_EVEN, DH_B), 0.1),
        'lambda_k2': nrm((N_EVEN, DH_B), 0.1),
        'subln_w': 1.0 + nrm((N_EVEN, 2 * DH_B), 0.02),
        'w_out_even': nrm((N_EVEN, D_MIX_EVEN, D_MODEL), beta * D_MIX_EVEN ** -0.5),
        'w_in_odd': nrm((N_ODD, D_MODEL, D_IN_ODD), D_MODEL ** -0.5),
        'b_gates_odd': b_gates_odd,
        'mh_norm_w': 1.0 + nrm((N_ODD, D_MIX_ODD), 0.02),
        'w_out_odd': nrm((N_ODD, D_MIX_ODD, D_MODEL), beta * D_MIX_ODD ** -0.5),
        'ln_mix_g': 1.0 + nrm((DEPTH, D_MODEL), 0.02),
        'ln_mix_b': nrm((DEPTH, D_MODEL), 0.02),
        'ln_ffn_g': 1.0 + nrm((DEPTH, D_MODEL), 0.02),
        'ln_ffn_b': nrm((DEPTH, D_MODEL), 0.02),
        'w_group': nrm((DEPTH, D_MODEL, N_GROUPS), D_MODEL ** -0.5),
        'w_router': nrm((DEPTH, D_MODEL, N_EXPERTS), D_MODEL ** -0.5),
        'w_exp_gate': nrm((DEPTH, N_EXPERTS, D_MODEL, D_EXPERT), D_MODEL ** -0.5),
        'w_exp_up': nrm((DEPTH, N_EXPERTS, D_MODEL, D_EXPERT), D_MODEL ** -0.5),
        'w_exp_down': nrm((DEPTH, N_EXPERTS, D_EXPERT, D_MODEL), beta * D_EXPERT ** -0.5),
        'w_ple_proj': nrm((DEPTH, D_PLE, D_MODEL), D_PLE ** -0.5),
        'w_ple_gate': nrm((DEPTH, D_MODEL, D_MODEL), D_MODEL ** -0.5),
    }


def reference(x_prompt, x_sample, cache_k, cache_v, page_table, state_conv, state_mlstm_C, state_mlstm_n,
              state_mlstm_m, p_prompt, p_sample, w_in_even, conv_w, lambda_q1, lambda_k1, lambda_q2, lambda_k2,
              subln_w, w_out_even, w_in_odd, b_gates_odd, mh_norm_w, w_out_odd, ln_mix_g, ln_mix_b, ln_ffn_g,
              ln_ffn_b, w_group, w_router, w_exp_gate, w_exp_up, w_exp_down, w_ple_proj, w_ple_gate):
    bp, tp, _ = x_prompt.shape
    bs, ts, _ = x_sample.shape
    past_len = page_table.shape[1] * cache_k.shape[2]
    pos_p = jnp.arange(tp)
    pos_s = past_len + jnp.arange(ts)
    kpos_s = jnp.arange(past_len + ts)
    xp, xs = x_prompt, x_sample
    kp_l, vp_l, cp_l, Cp_l, np_l, mp_l = [], [], [], [], [], []
    ks_l, vs_l, cs_l, Cs_l, ns_l, ms_l = [], [], [], [], [], []
    for i in range(DEPTH):
        j = i // 2
        if i % 2 == 0:
            lam_init = 0.8 - 0.6 * math.exp(-0.3 * i)
            lam = (jnp.exp(jnp.sum(lambda_q1[j] * lambda_k1[j]).astype(jnp.float32))
                   - jnp.exp(jnp.sum(lambda_q2[j] * lambda_k2[j]).astype(jnp.float32)) + lam_init)
            attend_p = lambda q, k, v: diff_attention_prompt(q, k, v, pos_p, lam)
            mix_p, k_new, v_new, c_new = even_mixer(xp, jnp.zeros((bp, CONV_W - 1, D_CONV), xp.dtype), pos_p,
                                                    attend_p, lam_init, w_in_even[j], conv_w[j], subln_w[j], w_out_even[j])
            kp_l.append(k_new); vp_l.append(v_new); cp_l.append(c_new)
            k_past = cache_k[j][page_table].reshape(bs, past_len, 2 * H_B, DH_B)
            v_past = cache_v[j][page_table].reshape(bs, past_len, H_B, 2 * DH_B)
            attend_s = lambda q, k, v: diff_attn_block(
                q, jnp.concatenate([k_past.astype(k.dtype), k], axis=1),
                jnp.concatenate([v_past.astype(v.dtype), v], axis=1), pos_s, kpos_s, lam)
            mix_s, k_new, v_new, c_new = even_mixer(xs, state_conv[j], pos_s, attend_s, lam_init,
                                                    w_in_even[j], conv_w[j], subln_w[j], w_out_even[j])
            ks_l.append(k_new); vs_l.append(v_new); cs_l.append(c_new)
        else:
            mix_p, C_new, n_new, m_new = odd_mixer(
                xp, jnp.zeros((bp, H_C, DK_C, DV_C), jnp.float32), jnp.zeros((bp, H_C, DK_C), jnp.float32),
                jnp.zeros((bp, H_C), jnp.float32), w_in_odd[j], b_gates_odd[j], mh_norm_w[j], w_out_odd[j])
            Cp_l.append(C_new); np_l.append(n_new); mp_l.append(m_new)
            mix_s, C_new, n_new, m_new = odd_mixer(xs, state_mlstm_C[j], state_mlstm_n[j], state_mlstm_m[j],
                                                   w_in_odd[j], b_gates_odd[j], mh_norm_w[j], w_out_odd[j])
            Cs_l.append(C_new); ns_l.append(n_new); ms_l.append(m_new)
        tail = (ln_mix_g[i], ln_mix_b[i], ln_ffn_g[i], ln_ffn_b[i], w_group[i], w_router[i],
                w_exp_gate[i], w_exp_up[i], w_exp_down[i], w_ple_proj[i], w_ple_gate[i])
        xp = layer_tail(xp, mix_p, p_prompt[i], *tail)
        xs = layer_tail(xs, mix_s, p_sample[i], *tail)
    return (xp, xs,
            jnp.stack(kp_l), jnp.stack(vp_l), jnp.stack(cp_l), jnp.stack(Cp_l), jnp.stack(np_l), jnp.stack(mp_l),
            jnp.stack(ks_l), jnp.stack(vs_l), jnp.stack(cs_l), jnp.stack(Cs_l), jnp.stack(ns_l), jnp.stack(ms_l))
```

```python
import numpy as np
from contextlib import ExitStack
import concourse.bass as bass
import concourse.mybir as mybir
from concourse.bass_utils import run_bass_kernel_spmd

F32 = mybir.dt.float32
BF16 = mybir.dt.bfloat16
I32 = mybir.dt.int32
AF = mybir.ActivationFunctionType
ALU = mybir.AluOpType
AX = mybir.AxisListType

CELL = 512
DBG = {}
ALPHA = 4.0 ** 0.25
LN_EPS = 1e-5
LAM_INIT0 = 0.8 - 0.6 * 1.0


class View:
    __slots__ = ("space", "ap", "lo", "hi", "base", "off", "esz", "shape")

    def __init__(self, space, ap, lo, hi, base=None, off=0, esz=4, shape=None):
        self.space = space
        self.ap = ap
        self.lo = lo
        self.hi = hi
        self.base = base if base is not None else ap
        self.off = off
        self.esz = esz
        self.shape = shape

    def __getitem__(self, idx):
        if not isinstance(idx, tuple):
            idx = (idx,)
        shape = self.shape
        strides = []
        s = 1
        for d in reversed(shape):
            strides.append(s)
            s *= d
        strides = strides[::-1]
        lo_e = 0
        hi_e = 0
        full = list(idx) + [slice(None)] * (len(shape) - len(idx))
        for ix, d, stv in zip(full, shape, strides):
            if isinstance(ix, int):
                a, b = ix, ix + 1
            else:
                a = 0 if ix.start is None else ix.start
                b = d if ix.stop is None else ix.stop
            assert 0 <= a < b <= d, (idx, shape)
            lo_e += a * stv
            hi_e += (b - 1) * stv
        ap = self.base[(slice(None),) + tuple(idx)]
        return View(self.space, ap, self.off + lo_e * self.esz, self.off + (hi_e + 1) * self.esz)

    def pp(self, p0, p1):
        return View(self.space, self.ap[p0:p1], self.lo, self.hi)

    def w(self, ap):
        return View(self.space, ap, self.lo, self.hi)


class Arena:
    def __init__(self, name, handle, nbytes):
        self.name = name
        self.t = handle
        self.nbytes = nbytes
        self.cur = 0
        self.handles = {}

    def view(self, off, shape, dt):
        esz = 2 if dt == BF16 else 4
        n = int(np.prod(shape))
        assert off % 4 == 0 and off + n * esz <= self.nbytes, (self.name, off, shape, self.nbytes)
        h = self.handles.get(dt)
        if h is None:
            h = self.t if dt == F32 else self.t.bitcast(dt)
            self.handles[dt] = h
        ap = h[:, off // esz: off // esz + n]
        if len(shape) == 2:
            ap = ap.rearrange("p (a b) -> p a b", a=shape[0])
        elif len(shape) == 3:
            ap = ap.rearrange("p (a b c) -> p a b c", a=shape[0], b=shape[1])
        elif len(shape) == 4:
            ap = ap.rearrange("p (a b c d) -> p a b c d", a=shape[0], b=shape[1], c=shape[2])
        return View(self.name, ap, off, off + n * esz, base=ap, off=off, esz=esz, shape=tuple(shape))

    def alloc(self, shape, dt):
        esz = 2 if dt == BF16 else 4
        n = int(np.prod(shape)) * esz
        n = (n + 63) // 64 * 64
        off = self.cur
        self.cur += n
        assert self.cur <= self.nbytes, (self.name, self.cur, self.nbytes)
        return self.view(off, shape, dt)


class Instr:
    __slots__ = ("eng", "idx", "fn", "waits", "signal", "sig", "dma", "dsem", "dval", "snap", "own", "inc", "tag")

    def __init__(self, eng, idx, fn, dma, inc):
        self.eng = eng
        self.idx = idx
        self.fn = fn
        self.dma = dma
        self.inc = inc
        self.waits = []
        self.signal = False
        self.sig = 0
        self.dsem = None
        self.dval = 0
        self.snap = None
        self.own = None


class Sched:
    ENGS = ("pe", "act", "dve", "pool", "sp")

    def __init__(self):
        self.streams = {e: [] for e in self.ENGS}
        self.clock = {e: {} for e in self.ENGS}
        self.snapc = {e: None for e in self.ENGS}
        self.n_dma_sems = {"sp": 30, "pool": 30, "act": 4, "pe": 1, "dve": 1}
        self.dma_rr = {e: 0 for e in self.ENGS}
        self.dma_tot = {}
        self.dma_last = {}
        self.cells = {}

    def _cells(self, v):
        if v.lo is None:
            return [(v.space, 0)]
        cell = 2048 if v.space == "ps" else CELL
        return [(v.space, c) for c in range(v.lo // cell, (v.hi - 1) // cell + 1)]

    def _merge(self, eng, dep):
        clk = self.clock[eng]
        ch = False
        for k, v in dep.snap.items():
            if clk.get(k, -1) < v:
                clk[k] = v
                ch = True
        k, v = dep.own
        if clk.get(k, -1) < v:
            clk[k] = v
            ch = True
        if ch:
            self.snapc[eng] = None

    def _need(self, ins, dep):
        if dep is None or dep is ins:
            return
        clk = self.clock[ins.eng]
        if dep.dma:
            if clk.get(("d", dep.dsem), 0) >= dep.dval:
                return
            ins.waits.append(("d", dep.dsem, dep.dval))
        else:
            if dep.eng == ins.eng and ins.eng == "pe" and not ins.dma:
                return
            if clk.get(("e", dep.eng), -1) >= dep.idx:
                return
            dep.signal = True
            ins.waits.append(("e", dep.eng, dep))
        self._merge(ins.eng, dep)

    def add(self, eng, fn, reads=(), writes=(), dma=False, inc=16, semkey=None):
        st = self.streams[eng]
        ins = Instr(eng, len(st), fn, dma, inc)
        import traceback as _tb
        ins.tag = [f.lineno for f in _tb.extract_stack(limit=6)][:-1] if DBG.get('names') is not None else None
        rc = []
        for v in reads:
            rc.extend(self._cells(v))
        wc = []
        for v in writes:
            wc.extend(self._cells(v))
        seen = set()
        for c in rc:
            ent = self.cells.get(c)
            if ent is not None and ent[0] is not None and id(ent[0]) not in seen:
                seen.add(id(ent[0]))
                self._need(ins, ent[0])
            if ent is not None and c[0] == "ps":
                for rd in reversed(ent[1]):
                    if rd.eng != ins.eng and id(rd) not in seen:
                        seen.add(id(rd))
                        self._need(ins, rd)
        for c in wc:
            ent = self.cells.get(c)
            if ent is None:
                continue
            if ent[0] is not None and id(ent[0]) not in seen:
                seen.add(id(ent[0]))
                self._need(ins, ent[0])
            for rd in reversed(ent[1]):
                if id(rd) in seen:
                    continue
                seen.add(id(rd))
                if rd.eng == ins.eng and ins.eng == "pe" and not rd.dma and not ins.dma:
                    continue
                self._need(ins, rd)
        if dma:
            if semkey is not None:
                k = (eng, semkey)
            else:
                k = (eng, self.dma_rr[eng] % self.n_dma_sems[eng])
                self.dma_rr[eng] += 1
            prev = self.dma_last.get(k)
            clk = self.clock[eng]
            if prev is not None and clk.get(("d", k), 0) < prev.dval:
                ins.waits.append(("d", k, prev.dval))
                self._merge(eng, prev)
            tot = self.dma_tot.get(k, 0) + inc
            self.dma_tot[k] = tot
            ins.dsem = k
            ins.dval = tot
            self.dma_last[k] = ins
            ins.own = (("d", k), tot)
        else:
            ins.own = (("e", eng), ins.idx)
        if self.snapc[eng] is None:
            self.snapc[eng] = dict(self.clock[eng])
        ins.snap = self.snapc[eng]
        for c in rc:
            ent = self.cells.get(c)
            if ent is None:
                ent = self.cells[c] = [None, []]
            if not ins.dma:
                ent[1] = [r for r in ent[1] if r.dma or r.eng != ins.eng]
            ent[1].append(ins)
        for c in wc:
            self.cells[c] = [ins, []]
        st.append(ins)
        return ins

    def emit(self, nc, stack):
        esem = {e: stack.enter_context(nc.semaphore("es_" + e)) for e in self.ENGS}
        dsem = {}
        for k in self.dma_tot:
            dsem[k] = stack.enter_context(nc.semaphore("ds_%s_%d" % k))
        for e in self.ENGS:
            c = 0
            for ins in self.streams[e]:
                if ins.signal:
                    c += 1
                    ins.sig = c
        block = stack.enter_context(nc.Block())
        final = [(dsem[k], v) for k, v in self.dma_tot.items()]

        def body(e, is_last_waiter):
            def run(engh):
                for ins in self.streams[e]:
                    for w in ins.waits:
                        if w[0] == "d":
                            engh.wait_ge(dsem[w[1]], w[2])
                        else:
                            engh.wait_ge(esem[w[1]], w[2].sig)
                    bi = ins.fn(engh)
                    if DBG.get('names') is not None:
                        try:
                            DBG['names'][str(bi.ins.name)] = (e, ins.idx, getattr(ins, 'tag', None))
                        except Exception as ex:
                            DBG['names']['err'] = str(ex)
                    if ins.dma:
                        bi.then_inc(dsem[ins.dsem], ins.inc)
                    elif ins.signal:
                        bi.then_inc(esem[e], 1)
                if is_last_waiter:
                    for s, v in final:
                        engh.wait_ge(s, v)
            return run

        block.sync(body("sp", True))
        block.tensor(body("pe", False))
        block.scalar(body("act", False))
        block.vector(body("dve", False))
        block.gpsimd(body("pool", False))


class U:
    __slots__ = ("ap",)

    def __init__(self, ap):
        self.ap = ap


def _tr(xs):
    return [x for x in xs if isinstance(x, View)]


class KB:
    def __init__(self, nc, S):
        self.nc = nc
        self.S = S

    def mm(self, out, lhsT, rhs, start=True, stop=True):
        self.S.add("pe", lambda e: e.matmul(out.ap, lhsT=lhsT.ap, rhs=rhs.ap, start=start, stop=stop),
                   reads=_tr([lhsT, rhs]), writes=[out])

    def tr(self, out, in_, ident):
        self.S.add("pe", lambda e: e.transpose(out.ap, in_.ap, ident.ap), reads=_tr([in_, ident]), writes=[out])

    def act(self, out, in_, func, bias=None, scale=None, accum=None):
        kw = {}
        rd = [in_]
        if bias is not None:
            kw["bias"] = bias.ap if isinstance(bias, View) else bias
            rd.append(bias)
        if scale is not None:
            kw["scale"] = scale.ap if isinstance(scale, View) else scale
            rd.append(scale)
        wr = [out]
        if accum is not None:
            kw["accum_out"] = accum.ap
            wr.append(accum)
        self.S.add("act", lambda e: e.activation(out=out.ap, in_=in_.ap, func=func, **kw), reads=_tr(rd), writes=wr)

    def tt(self, eng, out, a, b, op):
        self.S.add(eng, lambda e: e.tensor_tensor(out=out.ap, in0=a.ap, in1=b.ap, op=op), reads=_tr([a, b]), writes=[out])

    def ts(self, eng, out, a, s1, op0, s2=None, op1=None, accum=None):
        rd = [a, s1, s2]
        v1 = s1.ap if isinstance(s1, View) else s1
        v2 = s2.ap if isinstance(s2, View) else s2
        kw = {}
        wr = [out]
        if op1 is not None:
            kw["op1"] = op1
        if accum is not None:
            kw["accum_out"] = accum.ap
            wr.append(accum)
        self.S.add(eng, lambda e: e.tensor_scalar(out=out.ap, in0=a.ap, scalar1=v1, scalar2=v2, op0=op0, **kw),
                   reads=_tr(rd), writes=wr)

    def stt(self, eng, out, a, sc, b, op0, op1):
        v = sc.ap if isinstance(sc, View) else sc
        self.S.add(eng, lambda e: e.scalar_tensor_tensor(out=out.ap, in0=a.ap, scalar=v, in1=b.ap, op0=op0, op1=op1),
                   reads=_tr([a, sc, b]), writes=[out])

    def copy(self, eng, out, a):
        if eng == "act":
            self.act(out, a, AF.Copy)
        else:
            self.S.add(eng, lambda e: e.tensor_copy(out=out.ap, in_=a.ap), reads=_tr([a]), writes=[out])

    def red(self, eng, out, a, op):
        self.S.add(eng, lambda e: e.tensor_reduce(out=out.ap, in_=a.ap, axis=AX.X, op=op), reads=_tr([a]), writes=[out])

    def recip(self, out, a):
        self.S.add("dve", lambda e: e.reciprocal(out=out.ap, in_=a.ap), reads=_tr([a]), writes=[out])

    def memset(self, eng, out, val):
        self.S.add(eng, lambda e: e.memset(out.ap, val), writes=[out])

    def dma(self, q, out, in_, **kw):
        self.S.add(q, lambda e: e.dma_start(out=out.ap, in_=in_.ap, **kw), reads=_tr([in_]), writes=_tr([out]), dma=True)

    def asel(self, out, in_, pattern, op, fill, base, cm):
        self.S.add("pool", lambda e: e.affine_select(out=out.ap, in_=in_.ap, pattern=pattern, compare_op=op,
                                                      fill=fill, base=base, channel_multiplier=cm),
                   reads=_tr([in_]), writes=[out])


SB_BYTES = 211968
CONST_BYTES = 16896


def build(T=2048, NPG=64, NPOOL=2560, stop=99):
    NB = T // 128
    NT = T // 512
    TT = T + 32
    NG = NPG // 8
    GU = max(1, NG // 4)
    UPS = NG // GU
    nc = bass.Bass("TRN2", target_bir_lowering=False)
    S = Sched()
    K = KB(nc, S)
    st = ExitStack()
    st.enter_context(nc.allow_low_precision("bf16 matmul operands, fp32 accumulation"))
    ins = {}

    def din(name, shape, dt=F32):
        ins[name] = (tuple(shape), dt)
        return nc.dram_tensor(name, list(shape), dt, kind="ExternalInput").ap()

    def dout(name, shape):
        return nc.dram_tensor(name, list(shape), F32, kind="ExternalOutput").ap()

    d_xo = din("xo", [T, 1024]); d_xp = din("xp", [T, 1024]); d_xs = din("xs", [32, 1024]); d_xs16 = din("xs16", [16, 1024])
    d_pp = din("pp", [2, T, 256]); d_psm = din("psm", [2, 32, 256])
    d_poolk = din("poolk", [NPOOL, 128, 128]); d_poolv = din("poolv", [NPOOL, 128, 128])
    d_ptab = din("ptab", [8, 16 * NG], I32)
    d_sconv = din("sconv", [32, 1024])
    d_sC = din("sC", [32, 4, 128, 256]); d_sn = din("sn", [32, 4, 128]); d_sm = din("sm", [32, 4])
    d_wine = din("w_in_e", [1024, 3072]); d_wqs = din("w_qkv_s", [1024, 384])
    d_rows = din("rows", [77, 128]); d_lams = din("lams", [4, 64]); d_subrow = din("subrow", [1, 128])
    d_woe = din("w_out_e", [1024, 1024]); d_wino = din("w_in_o", [1024, 3080]); d_bg = din("b_gates", [1, 8])
    d_mhw = din("mhw", [1, 1024]); d_woo = din("w_out_o", [1024, 1024])
    d_wrt = din("w_rt", [2, 1024, 20])
    d_weg = din("w_eg", [2, 16, 1024, 512]); d_weu = din("w_eu", [2, 16, 1024, 512]); d_wed = din("w_ed", [2, 16, 512, 1024])
    d_wpp = din("w_pp", [2, 256, 1024]); d_wpg = din("w_pg", [2, 1024, 1024])
    d_ident = din("ident", [128, 128]); d_ropeo = din("rope_o", [T, 16]); d_ropep = din("rope_p", [T, 16]); d_ropes = din("rope_s", [32, 16])
    d_cfl = din("cflags", [128, 4]); d_sel16 = din("sel16", [16, 2048]); d_place = din("place", [16, 32]); d_hmask = din("hmask", [32, 4])
    d_m01 = din("m01", [2, 2]); d_e2 = din("e2", [2, 256]); d_tri = din("tri", [128, 128]); d_sel4 = din("sel4", [4, 512])
    o_yp = dout("y_p", [T, 1024]); o_ys = dout("y_s", [32, 1024]); o_kp = dout("k_p", [T, 512]); o_vp = dout("v_p", [T, 512])
    o_convp = dout("conv_p", [2, 512]); o_Cp = dout("C_p", [4, 128, 256]); o_np = dout("n_p", [4, 128]); o_mp = dout("m_p", [1, 4])
    o_ks = dout("k_s", [32, 512]); o_vs = dout("v_s", [32, 512]); o_convs = dout("conv_s", [32, 2, 512])
    o_Cs = dout("C_s", [32, 4, 128, 256]); o_ns = dout("n_s", [32, 4, 128]); o_ms = dout("m_s", [32, 4])
    cc_in = nc.dram_tensor("cc_in", [32, 512], F32)
    cc_out = nc.dram_tensor("cc_out", [32, 512], F32)
    st_in = nc.dram_tensor("st_in", [128, 1032], F32)
    st_out = nc.dram_tensor("st_out", [256, 1032], F32)
    V_ccin = View("d_ccin", cc_in.ap(), None, None)
    V_ccout = View("d_ccout", cc_out.ap(), None, None)
    V_stin = View("d_stin", st_in.ap(), None, None)
    V_stout = View("d_stout", st_out.ap(), None, None)

    sbt = st.enter_context(nc.sbuf_tensor("sb", [128, SB_BYTES // 4], F32))
    SBA = Arena("sb", sbt, SB_BYTES)
    SA_B = 8 * TT * 4
    SB_B = 8 * TT * 2
    C0 = 0
    SA0 = CONST_BYTES
    SB0 = SA0 + SA_B
    W0 = SB0 + SB_B
    WEND = SB_BYTES

    class Sub:
        def __init__(self, lo, hi):
            self.lo, self.hi, self.cur = lo, hi, lo

        def alloc(self, shape, dt):
            esz = 2 if dt == BF16 else 4
            n = (int(np.prod(shape)) * esz + 63) // 64 * 64
            off = self.cur
            self.cur += n
            assert self.cur <= self.hi, ("arena overflow", self.cur, self.hi, shape)
            return SBA.view(off, shape, dt)

    CA = Sub(C0, SA0)
    WK = Sub(W0, WEND)
    pst = [st.enter_context(nc.psum_tensor("ps%d" % b, [128, 512], F32)) for b in range(8)]
    PS = [View("ps", pst[b][:, :], b * 2048, (b + 1) * 2048, base=pst[b][:, :], off=b * 2048, esz=4, shape=(512,)) for b in range(8)]

    def SAv(i):
        w = 512 if i < NT else 32
        return SBA.view(SA0 + i * 16384, [8, w], F32)

    def SBv(i):
        w = 512 if i < NT else 32
        return SBA.view(SB0 + i * 8192, [8, w], BF16)

    def MTv(i):
        w = 512 if i < NT else 32
        return SBA.view(SA0 + SA_B - SB_B + i * 8192, [8, w], BF16)

    TILES = [(i, 512) for i in range(NT)] + [(NT, 32)]

    ident = CA.alloc([128], F32); K.dma("sp", ident, U(d_ident))
    identb = CA.alloc([128], BF16); K.dma("pool", identb, U(d_ident))
    ones_b = CA.alloc([128], BF16); K.memset("pool", ones_b, 1.0)
    ones_f = CA.alloc([128], F32); K.memset("pool", ones_f, 1.0)
    ones_f_off = ones_f.off
    cfl = CA.alloc([4], F32); K.dma("sp", cfl, U(d_cfl))
    rowsT = CA.alloc([80], F32)
    lnA = CA.alloc([64], F32)
    ropeo = CA.alloc([NB, 16], F32); K.dma("sp", ropeo, U(d_ropeo.rearrange("(b p) c -> p b c", p=128)))
    ropep = CA.alloc([NB, 16], F32); K.dma("sp", ropep, U(d_ropep.rearrange("(b p) c -> p b c", p=128)))
    ropes = CA.alloc([16], F32); K.dma("sp", ropes.pp(0, 32), U(d_ropes))
    sel16b = CA.alloc([16, 128], BF16); K.dma("pool", sel16b.pp(0, 16), U(d_sel16.rearrange("k (e m) -> k e m", e=16)))
    place = CA.alloc([32], F32); K.dma("sp", place.pp(0, 16), U(d_place))
    hmask = CA.alloc([4], F32); K.dma("sp", hmask.pp(0, 32), U(d_hmask))
    m01 = CA.alloc([2], F32); K.dma("sp", m01.pp(0, 2), U(d_m01))
    e2c = CA.alloc([16, 16], F32); K.dma("sp", e2c.pp(0, 2), U(d_e2.rearrange("k (i m) -> k i m", i=16)))
    tri = CA.alloc([128], F32); K.dma("sp", tri, U(d_tri))
    sel4 = CA.alloc([4, 128], F32); K.dma("sp", sel4.pp(0, 4), U(d_sel4.rearrange("k (e m) -> k e m", e=4)))
    subrow = CA.alloc([128], F32); K.dma("sp", subrow.pp(0, 16), U(d_subrow.partition_broadcast(16)))
    bgr = CA.alloc([8], F32); K.dma("sp", bgr, U(d_bg.partition_broadcast(128)))
    lamv = CA.alloc([4], F32)
    oattn = CA.alloc([512], F32)
    wr = CA.alloc([2, 8, 20], F32)
    for l in range(2):
        K.dma("sp", wr[l], U(d_wrt[l].rearrange("(k p) n -> p k n", p=128)))
    small = CA.alloc([64], F32)
    K.memset("dve", small[63:64], LN_EPS)
    WK.cur = W0
    tmpr = WK.alloc([128], F32)
    K.dma("sp", tmpr.pp(0, 77), U(d_rows))
    K.tr(PS[0][0:77], tmpr.pp(0, 77), ident[0:77].pp(0, 77))
    K.copy("dve", rowsT[0:77], PS[0][0:77])
    K.ts("dve", lnA, rowsT[0:64], ALPHA, ALU.mult)
    K.ts("dve", rowsT[76:77], rowsT[76:77], 1.0 - LAM_INIT0, ALU.mult)
    K.ts("dve", subrow.pp(0, 16), subrow.pp(0, 16), 1.0 - LAM_INIT0, ALU.mult)

    def lncol(l, j, c, alpha=False):
        i = l * 32 + j * 8 + c
        return (lnA if alpha else rowsT)[i:i + 1]

    def convcol(j, c):
        return rowsT[64 + j * 4 + c: 64 + j * 4 + c + 1]
    subcol = rowsT[76:77]
    lt = WK.alloc([4, 64], F32)
    K.dma("sp", lt, U(d_lams.partition_broadcast(128)))
    lp = WK.alloc([2, 64], F32)
    K.tt("dve", lp[0], lt[0], lt[1], ALU.mult)
    K.tt("dve", lp[1], lt[2], lt[3], ALU.mult)
    K.red("dve", small[0:2], lp, ALU.add)
    K.act(small[2:4], small[0:2], AF.Exp)
    K.tt("dve", small[4:5], small[2:3], small[3:4], ALU.subtract)
    K.ts("dve", lamv[0:1], small[4:5], LAM_INIT0, ALU.add)
    K.ts("dve", lamv[1:2], lamv[0:1], -1.0, ALU.mult)
    K.stt("dve", lamv[2:3].pp(0, 2), m01[1:2].pp(0, 2), lamv[0:1].pp(0, 2), m01[0:1].pp(0, 2), ALU.mult, ALU.add)
    neglam = lamv[1:2]
    if stop <= 0:
        S.emit(nc, st); st.close(); return nc, ins
    KT = SBA.view(SB0, [4, 2 * T], BF16)
    Vb = SBA.view(SA0, [2 * NB, 512], BF16)
    WK.cur = W0 + 1024
    wkv = WK.alloc([8, 1024], BF16)
    wqs = WK.alloc([8, 384], BF16)
    K.dma("pool", wkv, U(d_wine[:, 2048:3072].rearrange("(k p) n -> p k n", p=128)))
    K.dma("pool", wqs, U(d_wqs.rearrange("(k p) n -> p k n", p=128)))
    xstg = [WK.alloc([1024], F32) for _ in range(2)]
    xTb = [WK.alloc([8, 128], BF16) for _ in range(2)]
    kf = [WK.alloc([512], F32) for _ in range(2)]
    vf = [WK.alloc([512], F32) for _ in range(2)]
    rtmp = [WK.alloc([4, 64], F32) for _ in range(2)]

    def rope(xv, np_, cs, tmp, eng="dve", nh=8):
        x3 = xv.ap.rearrange("p (h d) -> p h d", h=nh)
        x1 = xv.w(x3[:, :, 0:8]); x2 = xv.w(x3[:, :, 8:16])
        cosb = cs.w(cs.ap[:, 0:8].unsqueeze(1).broadcast_to([np_, nh, 8]))
        sinb = cs.w(cs.ap[:, 8:16].unsqueeze(1).broadcast_to([np_, nh, 8]))
        t = [tmp.w(tmp.ap[0:np_, j, 0:nh * 8].rearrange("p (h d) -> p h d", h=nh)) for j in range(4)]
        K.tt(eng, t[0], x1, cosb, ALU.mult)
        K.tt(eng, t[1], x2, sinb, ALU.mult)
        K.tt(eng, t[2], x2, cosb, ALU.mult)
        K.tt(eng, t[3], x1, sinb, ALU.mult)
        K.tt(eng, x1, t[0], t[1], ALU.subtract)
        K.tt(eng, x2, t[2], t[3], ALU.add)

    def load_xT(src_rows, nrows, stg, dst, dst_is_f32_tile=None):
        K.dma("sp", stg.pp(0, nrows), U(src_rows))
        for half in range(2):
            b = PS[half]
            for c in range(4):
                kc = half * 4 + c
                K.tr(b[c * 128: c * 128 + nrows], stg[kc * 128:(kc + 1) * 128].pp(0, nrows), ident[0:nrows].pp(0, nrows))
        for half in range(2):
            src = PS[half].w(PS[half].ap.rearrange("p (c t) -> p c t", c=4)[:, :, 0:nrows])
            K.copy("act" if half == 0 else "dve", dst(half), src)

    def kv_block(src_rows, nrows, cs, slot, k_out, v_out, par, kt_dst=True):
        stg = xstg[par]; xt = xTb[par]
        load_xT(src_rows, nrows, stg, lambda half: xt.w(xt.ap[:, half * 4:(half + 1) * 4, 0:nrows]))
        for kc in range(8):
            K.mm(PS[2].pp(0, nrows), xt[kc, 0:nrows], wkv[kc, 0:512], start=(kc == 0), stop=(kc == 7))
        for kc in range(8):
            K.mm(PS[3].pp(0, nrows), xt[kc, 0:nrows], wkv[kc, 512:1024], start=(kc == 0), stop=(kc == 7))
        K.copy("act", kf[par].pp(0, nrows), PS[2].pp(0, nrows))
        if v_out is not None:
            K.copy("dve", vf[par].pp(0, nrows), PS[3].pp(0, nrows))
        if kt_dst:
            K.copy("act", Vb[slot].pp(0, nrows), (vf[par] if v_out is not None else PS[3]).pp(0, nrows))
        rope(kf[par].pp(0, nrows), nrows, cs, rtmp[par], eng="dve" if (par == 0 or DBG.get("ropedve")) else "pool")
        if k_out is not None:
            oq = DBG.get('outq', 'sp')
            if DBG.get('rows'):
                nr = DBG['rows']
                K.dma(oq, U(k_out[0:nr, :]), kf[par].pp(0, nr))
            elif DBG.get('srcx'):
                K.dma(oq, U(k_out), stg[0:512].pp(0, nrows))
            elif DBG.get('dsty'):
                K.dma(oq, U(o_yp[0:128, 0:512]), kf[par].pp(0, nrows))
            elif not DBG.get('nokout'):
                K.dma(oq, U(k_out), kf[par].pp(0, nrows))
            if not DBG.get('novout'):
                K.dma(oq, U(v_out), vf[par].pp(0, nrows))
        if kt_dst and not DBG.get('noKT'):
            for c in range(4):
                K.tr(PS[4][c * 128:(c + 1) * 128], kf[par][c * 128:(c + 1) * 128], ident)
            K.copy("act", KT[:, slot * 128:(slot + 1) * 128], PS[4].w(PS[4].ap.rearrange("p (c t) -> p c t", c=4)))

    xs16T = WK.alloc([8, 16], BF16)
    qkvf = WK.alloc([384], F32)
    q_hb = WK.alloc([128], BF16)
    pdiag = WK.alloc([16, 2], F32)
    vaugh = WK.alloc([129], F32)
    Kg = [WK.alloc([GU, 8, 128], F32) for _ in range(2)]
    Vg = [WK.alloc([GU, 8, 129], F32) for _ in range(2)]
    _prod = WK.alloc([GU * 8, 128], F32)
    Vt = SBA.view(_prod.off, [GU, 8 * 128], F32)
    prod = [_prod, _prod]
    sc = [WK.alloc([GU * 16], F32) for _ in range(2)]
    pe_ = [WK.alloc([GU * 8, 2], F32) for _ in range(2)]
    qbc = WK.alloc([128], F32)
    onb = [WK.alloc([128], F32) for _ in range(2)]
    coefE = WK.alloc([16, 16], F32)
    s1 = WK.alloc([160], F32)
    if not DBG.get('nosetup'):
        load_xT(d_xs16, 16, xstg[0], lambda half: xs16T.w(xs16T.ap[:, half * 4:(half + 1) * 4, :]))
        for kc in range(8):
            K.mm(PS[5][0:384].pp(0, 16), xs16T[kc], wqs[kc], start=(kc == 0), stop=(kc == 7))
        K.copy("act", qkvf.pp(0, 16), PS[5][0:384].pp(0, 16))
        rope(qkvf[0:256].pp(0, 16), 16, ropes.pp(0, 16), rtmp[0], nh=4)
        K.copy("dve", q_hb.pp(0, 16), qkvf[0:128].pp(0, 16))
        K.copy("dve", vaugh[0:128].pp(0, 16), qkvf[256:384].pp(0, 16))
        K.memset("dve", vaugh[128:129].pp(0, 16), 1.0)
        K.tt("dve", s1[0:128].pp(0, 16), qkvf[0:128].pp(0, 16), qkvf[128:256].pp(0, 16), ALU.mult)
        K.red("dve", s1[128:130].pp(0, 16), s1.w(s1.ap[0:16, 0:128].rearrange("p (s d) -> p s d", s=2)), ALU.add)
        K.act(s1[130:132].pp(0, 16), s1[128:130].pp(0, 16), AF.Exp, scale=0.125)
        K.tt("dve", pdiag.pp(0, 16),
             ident.w(ident.ap[0:16, 0:16].unsqueeze(2).broadcast_to([16, 16, 2])),
             s1.w(s1.ap[0:16, 130:132].unsqueeze(1).broadcast_to([16, 16, 2])), ALU.mult)
        K.ts("dve", coefE.pp(0, 2), e2c.pp(0, 2), lamv[2:3].pp(0, 2), ALU.mult)
        idxraw = WK.alloc([16 * NG], I32)
        idxv = WK.alloc([16 * NG], I32)
        for j8 in range(8):
            K.dma("sp", idxraw.pp(j8 * 16, j8 * 16 + 16), U(d_ptab[j8:j8 + 1, :].partition_broadcast(16)))
        K.ts("dve", idxv, idxraw, 16.0, ALU.mult, s2=cfl[2:3], op1=ALU.add)
        for b_ in range(2):
            K.memset("pool", Vg[b_].w(Vg[b_].ap[:, :, :, 128:129]), 1.0)
    pk_rows = d_poolk.rearrange("n (kg kk) f -> (n kg) (kk f)", kk=8)
    pv_rows = d_poolv.rearrange("n (kg kk) f -> (n kg) (kk f)", kk=8)

    def gather_unit(i, hh, par):
        for gl in range(GU):
            g = hh * GU + gl
            col = i * NG + g
            off = bass.IndirectOffsetOnAxis(ap=idxv[col:col + 1].ap, axis=0)
            ko = Kg[par][gl].ap.rearrange("p k f -> p (k f)")
            S.add("pool", lambda e, o=ko, off=off: e.indirect_dma_start(out=o, out_offset=None, in_=pk_rows, in_offset=off),
                  reads=[idxv[col:col + 1]], writes=[Kg[par][gl]], dma=True)
            S.add("pool", lambda e, o=Vt[gl].ap, off=off: e.indirect_dma_start(out=o, out_offset=None, in_=pv_rows, in_offset=off),
                  reads=[idxv[col:col + 1]], writes=[Vt[gl]], dma=True)
            K.copy("pool", Vg[par].w(Vg[par].ap[:, gl, :, 0:128]), Vt[gl].w(Vt[gl].ap.rearrange("p (k f) -> p k f", k=8)))

    def unit_compute(i, hh, par, uidx):
        eng = "dve" if uidx % 2 == 0 else "pool"
        if hh == 0:
            K.mm(PS[6][0:128], sel16b[i].pp(0, 16), q_hb.pp(0, 16))
            K.copy("act", qbc, PS[6][0:128])
        kg3 = Kg[par].w(Kg[par].ap.rearrange("p g k f -> p (g k) f"))
        K.tt(eng, prod[par], kg3, qbc.w(qbc.ap.unsqueeze(1).broadcast_to([128, GU * 8, 128])), ALU.mult)
        K.red("dve", sc[par], prod[par].w(prod[par].ap.rearrange("p a (s d) -> p (a s) d", s=2)), ALU.add)
        K.act(pe_[par].w(pe_[par].ap.rearrange("p a s -> p (a s)")), sc[par], AF.Exp, scale=0.125)
        for gl in range(GU):
            for kk in range(8):
                first = (hh == 0 and gl == 0 and kk == 0)
                K.mm(PS[5][0:129].pp(0, 2), pe_[par][gl * 8 + kk], Vg[par][gl, kk], start=first, stop=False)
        if hh == UPS - 1:
            K.mm(PS[5][0:129].pp(0, 2), pdiag[i].pp(0, 16), vaugh.pp(0, 16), start=False, stop=True)
            ob = onb[i % 2]
            K.recip(small[8:9].pp(0, 2), PS[5][128:129].pp(0, 2))
            K.ts("dve", ob.pp(0, 2), PS[5][0:128].pp(0, 2), small[8:9].pp(0, 2), ALU.mult)
            K.mm(PS[7][0:128].pp(0, 16), coefE[i].pp(0, 2), ob.pp(0, 2), start=(i == 0), stop=(i == 15))

    units = [(i, hh) for i in range(16) for hh in range(UPS)]
    blocks = [("pre", b) for b in range(NB)] + [("own", b) for b in range(NB)] + [("smp", 0)]
    if DBG.get('only'):
        blocks = [x for x in blocks if x[0] == DBG['only']][:DBG.get('nblk', 99)]
    if DBG.get('nosample'):
        units = []
    else:
        gather_unit(units[0][0], units[0][1], 0)
    nsteps = max(len(units), len(blocks))
    for step in range(nsteps):
        if step + 1 < len(units):
            gather_unit(units[step + 1][0], units[step + 1][1], (step + 1) % 2)
        if step < len(blocks) and not DBG.get('nokv'):
            kind, b = blocks[step]
            if kind == "pre":
                kv_block(d_xp[b * 128:(b + 1) * 128, :], 128, ropep[b], b, None, None, step % 2)
            elif kind == "own":
                if DBG.get('noout'):
                    kv_block(d_xo[b * 128:(b + 1) * 128, :], 128, ropeo[b], NB + b, None, None, step % 2)
                elif DBG.get('xpsrc'):
                    kv_block(d_xp[b * 128:(b + 1) * 128, :], 128, ropeo[b], NB + b,
                             o_kp[b * 128:(b + 1) * 128, :], o_vp[b * 128:(b + 1) * 128, :], step % 2)
                else:
                    kv_block(d_xo[b * 128:(b + 1) * 128, :], 128, ropeo[b], NB + b,
                             o_kp[b * 128:(b + 1) * 128, :], o_vp[b * 128:(b + 1) * 128, :], step % 2)
            else:
                if DBG.get('smpkp'):
                    kv_block(d_xs, 32, ropes.pp(0, 32), None, o_kp[0:32, :], o_vp[0:32, :], step % 2, kt_dst=False)
                elif DBG.get('smpkt'):
                    kv_block(d_xs, 32, ropes.pp(0, 32), NB, o_ks, o_vs, step % 2, kt_dst=True)
                else:
                    kv_block(d_xs, 32, ropes.pp(0, 32), None, o_ks, o_vs, step % 2, kt_dst=False)
        if step < len(units):
            unit_compute(units[step][0], units[step][1], step % 2, step)
    if DBG.get('nosample') or DBG.get('noepi'):
        S.emit(nc, st); st.close(); return nc, ins
    osb = SBA.view(_prod.off, [128], F32)
    obuf = SBA.view(_prod.off + 512, [4, 128], F32)
    K.copy("dve", osb.pp(0, 16), PS[7][0:128].pp(0, 16))
    K.act(s1[0:128].pp(0, 16), osb.pp(0, 16), AF.Square, accum=small[10:11].pp(0, 16))
    K.act(small[11:12].pp(0, 16), small[10:11].pp(0, 16), AF.Sqrt, scale=1.0 / 128.0, bias=small[63:64].pp(0, 16))
    K.recip(small[12:13].pp(0, 16), small[11:12].pp(0, 16))
    K.stt("dve", osb.pp(0, 16), osb.pp(0, 16), small[12:13].pp(0, 16), subrow.pp(0, 16), ALU.mult, ALU.mult)
    K.mm(PS[6][0:128].pp(0, 32), place.pp(0, 16), osb.pp(0, 16))
    for hq in range(4):
        K.ts("dve", obuf[hq].pp(0, 32), PS[6][0:128].pp(0, 32), hmask[hq:hq + 1].pp(0, 32), ALU.mult)
    K.dma("sp", V_ccin, obuf.w(obuf.ap[0:32].rearrange("p h e -> p (h e)")))
    S.add("pool", lambda e: e.collective_compute("AllReduce", ALU.add, replica_groups=[list(range(8))],
                                                 ins=[cc_in.ap().opt()], outs=[cc_out.ap().opt()]),
          reads=[V_ccin], writes=[V_ccout], dma=True, inc=1, semkey=100)
    K.dma("sp", oattn.pp(0, 32), V_ccout)
    if stop <= 1:
        S.emit(nc, st); st.close(); return nc, ins
    WK.cur = W0 + 1024
    wcq = WK.alloc([8, 2048], BF16)
    K.dma("pool", wcq[:, 0:1024], U(d_wine[:, 0:1024].rearrange("(k p) n -> p k n", p=128)))
    K.dma("pool", wcq[:, 1024:2048], U(d_wine[:, 1024:2048].rearrange("(k p) n -> p k n", p=128)))
    xstg = [WK.alloc([1024], F32) for _ in range(2)]
    xTt = WK.alloc([8, 512], BF16)
    Ub = WK.alloc([4, 514], F32)
    gcs = WK.alloc([512], F32)
    gbs = WK.alloc([4, 512], F32)
    cacc = WK.alloc([512], F32)
    qf = WK.alloc([512], F32)
    QT = WK.alloc([4, 512], BF16)
    Pt = [WK.alloc([512], BF16) for _ in range(4)]
    rl = [WK.alloc([512], F32) for _ in range(2)]
    tq = [WK.alloc([512], F32) for _ in range(2)]
    osq = WK.alloc([512], BF16)
    rtmp2 = WK.alloc([4, 64], F32)
    scT = WK.alloc([8, 32], F32)
    ustg = WK.alloc([512], F32)

    def conv_chunks(w, mt, first_cols_view=None, sample=False):
        for c in range(4):
            for j, (col0, bank) in enumerate(((512 + c * 128, 0), (1024 + c * 128, 1), (c * 128, 2))):
                for kc in range(8):
                    K.mm(PS[bank][0:w], wcq[kc, col0:col0 + 128], xTt[kc, 0:w], start=(kc == 0), stop=(kc == 7))
            K.copy("act", gcs[0:w], PS[0][0:w])
            K.tt("dve", Ub[c, 2:2 + w], PS[1][0:w], gcs[0:w], ALU.mult)
            K.copy("act", gbs[c, 0:w], PS[2][0:w])
            if not sample:
                K.ts("pool", cacc[0:w], Ub[c, 0:w], convcol(0, c), ALU.mult)
                K.stt("dve", cacc[0:w], Ub[c, 1:1 + w], convcol(1, c), cacc[0:w], ALU.mult, ALU.add)
                K.stt("dve", cacc[0:w], Ub[c, 2:2 + w], convcol(2, c), cacc[0:w], ALU.mult, ALU.add)
            else:
                K.ts("pool", cacc[0:w], scT[c], convcol(0, c), ALU.mult)
                K.stt("dve", cacc[0:w], scT[4 + c], convcol(1, c), cacc[0:w], ALU.mult, ALU.add)
                K.stt("dve", cacc[0:w], Ub[c, 2:2 + w], convcol(2, c), cacc[0:w], ALU.mult, ALU.add)
            K.tt("pool", mt[c], gbs[c, 0:w], cacc[0:w], ALU.mult)
            if not sample:
                K.copy("pool", Ub[c, 0:2], Ub[c, 512:514])

    load_xT(d_xp[T - 128:T, :], 128, xstg[0], lambda half: xTt.w(xTt.ap[:, half * 4:(half + 1) * 4, 0:128]))
    for c in range(4):
        for j, (col0, bank) in enumerate(((512 + c * 128, 0), (1024 + c * 128, 1))):
            for kc in range(8):
                K.mm(PS[bank][0:128], wcq[kc, col0:col0 + 128], xTt[kc, 0:128], start=(kc == 0), stop=(kc == 7))
        K.copy("act", gcs[0:128], PS[0][0:128])
        K.tt("dve", cacc[0:128], PS[1][0:128], gcs[0:128], ALU.mult)
        K.ts("dve", Ub[c, 0:2], cacc[126:128], cfl[0:1], ALU.mult)

    def attention(i, mt):
        nown = 4 * i + 4
        kblocks = [("pre", kb, 0) for kb in range(NB)] + [("own", kb, max(0, kb - 4 * i) * 128) for kb in range(nown)]
        for h in range(4):
            for j in range(2):
                Ob, Lb = PS[2 * j], PS[2 * j + 1]
                seq = []
                for n_, (kind, kb, qoff) in enumerate(kblocks):
                    col = (kb if kind == "pre" else NB + kb) * 128
                    seq.append((kind, kb, qoff, col, n_))

                def s_step(item):
                    kind, kb, qoff, col, n_ = item
                    nq = 512 - qoff
                    sb = PS[4 + n_ % 3]
                    K.mm(sb[0:nq], KT[h, col:col + 128].pp(j * 64, j * 64 + 64), QT[h, qoff:512].pp(j * 64, j * 64 + 64))
                    pt = Pt[n_ % 4]
                    K.act(pt[0:nq], sb[0:nq], AF.Exp, scale=0.125, bias=(cfl[1:2] if kind == "pre" else None))
                    if kind == "own" and kb >= 4 * i:
                        K.asel(pt[0:128], pt[0:128], [[1, 128]], ALU.is_ge, 0.0, 0, -1)

                def av_step(item, first, last):
                    kind, kb, qoff, col, n_ = item
                    nq = 512 - qoff
                    pt = Pt[n_ % 4]
                    slot = kb if kind == "pre" else NB + kb
                    K.mm(Ob[qoff:512], Vb[slot, h * 128:(h + 1) * 128], pt[0:nq], start=first, stop=last)
                    K.mm(Lb[qoff:512], ones_b, pt[0:nq], start=first, stop=last)
                s_step(seq[0])
                for n_ in range(len(seq)):
                    if n_ + 1 < len(seq):
                        s_step(seq[n_ + 1])
                    av_step(seq[n_], n_ == 0, n_ == len(seq) - 1)
                K.recip(rl[j], Lb)
                K.tt("dve", tq[j], Ob, rl[j], ALU.mult)
            K.stt("dve", tq[0], tq[1], neglam, tq[0], ALU.mult, ALU.add)
            K.tt("pool", osq, tq[0], tq[0], ALU.mult)
            K.mm(PS[7], ones_b, osq)
            K.act(rl[0], PS[7], AF.Sqrt, scale=1.0 / 128.0, bias=small[63:64])
            K.recip(rl[1], rl[0])
            K.stt("dve", mt[4 + h], tq[0], subcol, rl[1], ALU.mult, ALU.mult)

    for i in range(NT):
        mt = MTv(i)
        for blk in range(4):
            gb_ = i * 4 + blk
            load_xT(d_xo[gb_ * 128:(gb_ + 1) * 128, :], 128, xstg[blk % 2],
                    lambda half, blk=blk: xTt.w(xTt.ap[:, half * 4:(half + 1) * 4, blk * 128:(blk + 1) * 128]))
            for kc in range(8):
                K.mm(PS[2], xTt[kc, blk * 128:(blk + 1) * 128], wcq[kc, 1536:2048], start=(kc == 0), stop=(kc == 7))
            K.copy("act", qf, PS[2])
            rope(qf, 128, ropeo[gb_], rtmp2, eng="dve")
            for c in range(4):
                K.tr(PS[3][c * 128:(c + 1) * 128], qf[c * 128:(c + 1) * 128], ident)
            K.copy("act", QT[:, blk * 128:(blk + 1) * 128], PS[3].w(PS[3].ap.rearrange("p (c t) -> p c t", c=4)))
        conv_chunks(512, mt)
        attention(i, mt)
    for c in range(4):
        K.tr(PS[0][c * 128:(c + 1) * 128].pp(0, 2), Ub[c, 0:2], ident)
    K.copy("dve", ustg.pp(0, 2), PS[0].pp(0, 2))
    K.dma("sp", U(o_convp), ustg.pp(0, 2))
    mts = MTv(NT)
    load_xT(d_xs, 32, xstg[0], lambda half: xTt.w(xTt.ap[:, half * 4:(half + 1) * 4, 0:32]))
    load_xT(d_sconv, 32, xstg[1], lambda half: scT.w(scT.ap[:, half * 4:(half + 1) * 4, :]))
    K.dma("sp", U(o_convs[:, 0, :]), xstg[1][512:1024].pp(0, 32))
    conv_chunks(32, mts, sample=True)
    for c in range(4):
        K.tr(PS[0][c * 128:(c + 1) * 128].pp(0, 32), Ub[c, 2:34], ident)
    K.copy("dve", ustg.pp(0, 32), PS[0].pp(0, 32))
    K.dma("sp", U(o_convs[:, 1, :]), ustg.pp(0, 32))
    for c in range(4):
        K.tr(PS[1][c * 32:(c + 1) * 32], oattn[c * 128:(c + 1) * 128].pp(0, 32), ident[0:32].pp(0, 32))
    K.copy("act", mts.w(mts.ap[:, 4:8, :]), PS[1].w(PS[1].ap[:, 0:128].rearrange("p (c t) -> p c t", c=4)))
    if stop <= 2:
        S.emit(nc, st); st.close(); return nc, ins
    def layer_norm_tile(sa, sb, w, l, jg, alpha_out, lt, src=None):
        src = sa if src is None else src
        zb, zsq, mean, msq, var, t1_, t2_ = lt
        for c in range(8):
            K.copy("act", zb[c, 0:w], src[c])
            K.tt("pool", zsq[c, 0:w], src[c], src[c], ALU.mult)
        for c in range(8):
            K.mm(PS[6][0:w], ones_b, zb[c, 0:w], start=(c == 0), stop=(c == 7))
        for c in range(8):
            K.mm(PS[7][0:w], ones_b, zsq[c, 0:w], start=(c == 0), stop=(c == 7))
        K.act(mean[0:w], PS[6][0:w], AF.Copy, scale=1.0 / 1024.0)
        K.tt("pool", msq[0:w], mean[0:w], mean[0:w], ALU.mult)
        K.stt("dve", var[0:w], PS[7][0:w], 1.0 / 1024.0, msq[0:w], ALU.mult, ALU.subtract)
        K.act(var[0:w], var[0:w], AF.Sqrt, bias=small[63:64])
        K.recip(var[0:w], var[0:w])
        for c in range(8):
            tt_ = t1_[c % 2]
            K.tt("dve", tt_[0:w], src[c], mean[0:w], ALU.subtract)
            K.tt("pool", tt_[0:w], tt_[0:w], var[0:w], ALU.mult)
            K.act(sb[c], tt_[0:w], AF.Identity, scale=lncol(l, jg, c), bias=lncol(l, jg + 1, c))
            K.act(sa[c], tt_[0:w], AF.Identity, scale=lncol(l, jg, c, alpha_out), bias=lncol(l, jg + 1, c, alpha_out))

    def route_block(l, sa, c0, nr, gT, col0, rt):
        lg, ge, gx, oh, elm, els, ee, mk, wj, gate = rt
        for kc in range(8):
            K.mm(PS[5][0:20].pp(0, nr), sa[kc, c0:c0 + nr], wr[l, kc], start=(kc == 0), stop=(kc == 7))
        K.act(lg.pp(0, nr), PS[5][0:20].pp(0, nr), AF.Copy, scale=1.0 / ALPHA)
        P_ = lambda v: v.pp(0, nr)
        K.red("dve", P_(gx[0:1]), P_(lg[0:4]), ALU.max)
        K.ts("dve", P_(gx[1:2]), P_(gx[0:1]), -1.0, ALU.mult)
        K.act(P_(ge[0:4]), P_(lg[0:4]), AF.Exp, bias=P_(gx[1:2]), accum=P_(gx[2:3]))
        K.recip(P_(gx[3:4]), P_(gx[2:3]))
        K.ts("dve", P_(oh), P_(lg[0:4]), P_(gx[0:1]), ALU.is_equal)
        K.tt("dve", P_(elm), lg.w(lg.ap[0:nr, 4:20].rearrange("p (g j) -> p g j", g=4)),
             oh.w(oh.ap[0:nr].unsqueeze(2).broadcast_to([nr, 4, 4])), ALU.mult)
        K.red("dve", P_(els), elm.w(elm.ap[0:nr].rearrange("p g j -> p j g")), ALU.add)
        K.red("dve", P_(gx[4:5]), P_(els), ALU.max)
        K.ts("dve", P_(gx[5:6]), P_(gx[4:5]), -1.0, ALU.mult)
        K.act(P_(ee), P_(els), AF.Exp, bias=P_(gx[5:6]))
        K.red("dve", P_(gx[6:7]), P_(ee), ALU.max)
        K.ts("dve", P_(mk[0]), P_(ee), P_(gx[6:7]), ALU.is_equal)
        K.stt("dve", P_(mk[1]), P_(mk[0]), -2.0, P_(ee), ALU.mult, ALU.add)
        K.red("dve", P_(gx[7:8]), P_(mk[1]), ALU.max)
        K.ts("dve", P_(mk[2]), P_(mk[1]), P_(gx[7:8]), ALU.is_equal)
        K.tt("dve", P_(mk[0]), P_(mk[0]), P_(mk[2]), ALU.add)
        K.tt("dve", P_(gx[8:9]), P_(gx[6:7]), P_(gx[7:8]), ALU.add)
        K.recip(P_(gx[9:10]), P_(gx[8:9]))
        K.tt("dve", P_(gx[10:11]), P_(gx[9:10]), P_(gx[3:4]), ALU.mult)
        K.stt("dve", P_(wj), P_(ee), P_(gx[10:11]), P_(mk[0]), ALU.mult, ALU.mult)
        K.tt("dve", P_(gate), oh.w(oh.ap[0:nr].unsqueeze(2).broadcast_to([nr, 4, 4])),
             wj.w(wj.ap[0:nr].unsqueeze(1).broadcast_to([nr, 4, 4])), ALU.mult)
        K.tr(PS[5][32:32 + nr].pp(0, 16), gate.w(gate.ap[0:nr].rearrange("p g j -> p (g j)")), ident[0:nr].pp(0, nr))
        K.copy("act", gT[col0:col0 + nr].pp(0, 16), PS[5][32:32 + nr].pp(0, 16))

    def tail(l, d_wout, mt_of, x_loader):
        WK.cur = W0
        gT = WK.alloc([TT], BF16)
        rt = (WK.alloc([20], F32), WK.alloc([4], F32), WK.alloc([12], F32), WK.alloc([4], F32), WK.alloc([4, 4], F32),
              WK.alloc([4], F32), WK.alloc([4], F32), WK.alloc([3, 4], F32), WK.alloc([4], F32), WK.alloc([4, 4], F32))
        mark = WK.cur
        wout = WK.alloc([8, 1024], BF16)
        K.dma("pool", wout, U(d_wout.rearrange("(k p) n -> p k n", p=128)))
        lt = (WK.alloc([8, 512], BF16), WK.alloc([8, 512], BF16), WK.alloc([512], F32), WK.alloc([512], F32),
              WK.alloc([512], F32), [WK.alloc([512], F32) for _ in range(2)], None)
        wb = []
        xst = [WK.alloc([1024], F32) for _ in range(2)] if x_loader is not None else None
        xtmp = WK.alloc([8, 512], F32) if x_loader is not None else None
        for (i, w) in TILES:
            sa, sb, mt = SAv(i), SBv(i), mt_of(i)
            zs = sa
            if x_loader is not None:
                zs = SBA.view(xtmp.off, [8, w], F32)
                x_loader(i, w, zs, xst)
            for n in range(8):
                pb = PS[n % 4]
                for kc in range(8):
                    K.mm(pb[0:w], wout[kc, n * 128:(n + 1) * 128], mt[kc], start=(kc == 0), stop=(kc == 7))
                K.stt("dve", zs[n], zs[n], ALPHA, pb[0:w], ALU.mult, ALU.add)
            layer_norm_tile(sa, sb, w, l, 0, True, lt, src=zs)
            for blk in range((w + 127) // 128):
                nr = min(128, w - blk * 128)
                route_block(l, sa, blk * 128, nr, gT, i * 512 + blk * 128, rt)
        WK.cur = mark
        ebuf = [(WK.alloc([8, 512], BF16), WK.alloc([8, 512], BF16), WK.alloc([4, 1024], BF16)) for _ in range(2)]
        gbs_ = [WK.alloc([512], F32) for _ in range(2)]
        sg_ = [WK.alloc([512], F32) for _ in range(2)]
        t1b = [WK.alloc([512], F32) for _ in range(2)]
        hid = [WK.alloc([4, 512], BF16) for _ in range(2)]
        wpg = WK.alloc([8, 1024], BF16)
        wpp = WK.alloc([2, 1024], BF16)

        def load_expert(e):
            eg, eu, ed = ebuf[e % 2]
            K.dma("pool", eg, U(d_weg[l, e].rearrange("(k p) n -> p k n", p=128)))
            K.dma("pool", eu, U(d_weu[l, e].rearrange("(k p) n -> p k n", p=128)))
            K.dma("pool", ed, U(d_wed[l, e].rearrange("(k p) n -> p k n", p=128)))
        load_expert(0)
        work = [(e, i, w) for e in range(16) for (i, w) in TILES]

        def gu_step(k_):
            e, i, w = work[k_]
            eg, eu, ed = ebuf[e % 2]
            sb = SBv(i)
            par = k_ % 2
            K.mm(PS[6][0:w], sel16b[e].pp(0, 16), gT[i * 512:i * 512 + w].pp(0, 16))
            K.copy("act", gbs_[par][0:w], PS[6][0:w])
            for fc in range(4):
                pg, pu = PS[(2 * fc) % 4], PS[(2 * fc + 1) % 4]
                for kc in range(8):
                    K.mm(pg[0:w], eg[kc, fc * 128:(fc + 1) * 128], sb[kc], start=(kc == 0), stop=(kc == 7))
                for kc in range(8):
                    K.mm(pu[0:w], eu[kc, fc * 128:(fc + 1) * 128], sb[kc], start=(kc == 0), stop=(kc == 7))
                K.act(sg_[fc % 2][0:w], pg[0:w], AF.Silu)
                K.tt("dve", t1b[fc % 2][0:w], pu[0:w], gbs_[par][0:w], ALU.mult)
                K.tt("pool", hid[par][fc, 0:w], sg_[fc % 2][0:w], t1b[fc % 2][0:w], ALU.mult)

        def y_step(k_):
            e, i, w = work[k_]
            eg, eu, ed = ebuf[e % 2]
            sa = SAv(i)
            par = k_ % 2
            for n in range(8):
                pb = PS[4 + n % 2]
                for fc in range(4):
                    K.mm(pb[0:w], ed[fc, n * 128:(n + 1) * 128], hid[par][fc, 0:w], start=(fc == 0), stop=(fc == 3))
                K.tt("dve", sa[n], sa[n], pb[0:w], ALU.add)
        for k_ in range(len(work)):
            e, i, w = work[k_]
            if i == 0 and e + 1 < 16:
                load_expert(e + 1)
            if k_ == 0:
                gu_step(0)
            if k_ + 1 < len(work):
                gu_step(k_ + 1)
            y_step(k_)
            if k_ == 8:
                K.dma("pool", wpg, U(d_wpg[l].rearrange("(k p) n -> p k n", p=128)))
                K.dma("pool", wpp, U(d_wpp[l].rearrange("(k p) n -> p k n", p=128)))
        WK.cur = mark
        ltb = (WK.alloc([8, 512], BF16), WK.alloc([8, 512], BF16), WK.alloc([512], F32), WK.alloc([512], F32),
               WK.alloc([512], F32), [WK.alloc([512], F32) for _ in range(2)], None)
        assert WK.cur <= ebuf[1][0].off or True
        pstg = [SBA.view(gbs_[0].off, [256], F32), SBA.view(sg_[0].off, [256], F32)]
        pT = SBA.view(hid[0].off, [2, 512], BF16)
        sgm = [t1b[0], t1b[1]]
        tpl = [SBA.view(hid[1].off, [512], F32)]
        for (i, w) in TILES:
            sa, sb = SAv(i), SBv(i)
            layer_norm_tile(sa, sb, w, l, 2, False, ltb)
            for blk in range((w + 127) // 128):
                nr = min(128, w - blk * 128)
                src = d_pp[l, i * 512 + blk * 128: i * 512 + blk * 128 + nr, :] if i < NT else d_psm[l]
                K.dma("sp", pstg[blk % 2].pp(0, nr), U(src))
                for pc in range(2):
                    K.tr(PS[6][pc * 128: pc * 128 + nr], pstg[blk % 2][pc * 128:(pc + 1) * 128].pp(0, nr), ident[0:nr].pp(0, nr))
                K.copy("act", pT.w(pT.ap[:, :, blk * 128: blk * 128 + nr]),
                       PS[6].w(PS[6].ap[:, 0:256].rearrange("p (c t) -> p c t", c=2)[:, :, 0:nr]))
            for n in range(8):
                pa, pb = PS[(2 * n) % 4], PS[(2 * n + 1) % 4]
                for kc in range(8):
                    K.mm(pa[0:w], wpg[kc, n * 128:(n + 1) * 128], sb[kc], start=(kc == 0), stop=(kc == 7))
                for pc in range(2):
                    K.mm(pb[0:w], wpp[pc, n * 128:(n + 1) * 128], pT[pc, 0:w], start=(pc == 0), stop=(pc == 1))
                K.act(sgm[n % 2][0:w], pa[0:w], AF.Sigmoid)
                K.tt("dve", sgm[n % 2][0:w], pb[0:w], sgm[n % 2][0:w], ALU.mult)
                K.tt("pool", sa[n], sa[n], sgm[n % 2][0:w], ALU.add)
            for n in range(8):
                K.copy("act", sb[n], sa[n])

    def x_loader0(i, w, sa, xst):
        if i < NT:
            for blk in range(4):
                stg = xst[blk % 2]
                gb_ = i * 4 + blk
                K.dma("sp", stg, U(d_xo[gb_ * 128:(gb_ + 1) * 128, :]))
                for half in range(2):
                    for c in range(4):
                        K.tr(PS[4 + half][c * 128:(c + 1) * 128], stg[(half * 4 + c) * 128:(half * 4 + c + 1) * 128], ident)
                for half in range(2):
                    K.copy("act" if half == 0 else "dve", sa.w(sa.ap[:, half * 4:(half + 1) * 4, blk * 128:(blk + 1) * 128]),
                           PS[4 + half].w(PS[4 + half].ap.rearrange("p (c t) -> p c t", c=4)))
        else:
            stg = xst[0]
            K.dma("sp", stg.pp(0, 32), U(d_xs))
            for half in range(2):
                for c in range(4):
                    K.tr(PS[4 + half][c * 128:c * 128 + 32], stg[(half * 4 + c) * 128:(half * 4 + c + 1) * 128].pp(0, 32), ident[0:32].pp(0, 32))
            for half in range(2):
                K.copy("act" if half == 0 else "dve", sa.w(sa.ap[:, half * 4:(half + 1) * 4, :]),
                       PS[4 + half].w(PS[4 + half].ap.rearrange("p (c t) -> p c t", c=4)[:, :, 0:32]))

    tail(0, d_woe, MTv, x_loader0)
    if stop <= 3:
        S.emit(nc, st); st.close(); return nc, ins
    DKS = 128.0 ** -0.5
    NB4 = NB * 4
    WK.cur = W0
    wino = WK.alloc([8, 3080], BF16)
    K.dma("pool", wino[:, 0:1540], U(d_wino[:, 0:1540].rearrange("(k p) n -> p k n", p=128)))
    K.dma("pool", wino[:, 1540:3080], U(d_wino[:, 1540:3080].rearrange("(k p) n -> p k n", p=128)))
    mhw = WK.alloc([1024], F32); K.dma("sp", mhw, U(d_mhw.partition_broadcast(128)))
    IG = WK.alloc([NB4], F32); LF = WK.alloc([NB4], F32); BC = WK.alloc([NB4], F32); Aa = WK.alloc([NB4], F32)
    GS = WK.alloc([NB4], F32); ENM = WK.alloc([NB4], F32); WL = WK.alloc([NB4], F32); GL = WK.alloc([NB4], F32)
    MMt = WK.alloc([NB4], F32); rep = WK.alloc([NB4], F32); tmpc = WK.alloc([NB4], F32)
    rowA = WK.alloc([128], F32); rowB = WK.alloc([128], F32)
    r0 = WK.alloc([3, NB4 + 4], F32)
    Cf = WK.alloc([4, 257], F32); Cb = WK.alloc([4, 257], BF16)
    QTl = WK.alloc([4, 512], BF16); KTl = WK.alloc([4, 512], BF16)
    Ktm = WK.alloc([512], BF16); Va = WK.alloc([4, 257], BF16); Kw = WK.alloc([128], BF16)
    Et = WK.alloc([128], F32); SWt = WK.alloc([128], BF16); mmr = WK.alloc([128], F32)
    App = WK.alloc([256], F32); Hh = WK.alloc([1024], F32); sg1 = WK.alloc([1024], F32)
    sm_ = WK.alloc([32], F32); bst = WK.alloc([8], F32)
    K.memset("pool", Va.w(Va.ap[:, :, 256:257]), 1.0)
    for gb_ in range(NB):
        i, blk = gb_ // 4, gb_ % 4
        sb = SBv(i)
        for kc in range(8):
            K.mm(PS[0][0:8], sb[kc, blk * 128:(blk + 1) * 128], wino[kc, 3072:3080], start=(kc == 0), stop=(kc == 7))
        K.tt("dve", IG[gb_ * 4:gb_ * 4 + 4], PS[0][0:4], bgr[0:4], ALU.add)
        K.tt("dve", LF[gb_ * 4:gb_ * 4 + 4], PS[0][4:8], bgr[4:8], ALU.add)
    K.act(LF, LF, AF.Exp, scale=-1.0)
    K.act(LF, LF, AF.Ln, bias=1.0)
    K.ts("dve", LF, LF, -1.0, ALU.mult)
    K.mm(PS[0][0:NB4], tri, LF)
    K.copy("dve", BC, PS[0][0:NB4])
    K.tt("dve", Aa, IG, BC, ALU.subtract)
    K.tr(PS[1][0:128].pp(0, NB4), Aa, ident)
    K.copy("dve", rowA.pp(0, NB4), PS[1][0:128].pp(0, NB4))
    S.add("dve", lambda e: e.tensor_tensor_scan(out=rowB.pp(0, NB4).ap, data0=rowA.pp(0, NB4).ap, data1=rowA.pp(0, NB4).ap,
                                                initial=-1e30, op0=ALU.max, op1=ALU.max),
          reads=[rowA], writes=[rowB])
    K.tr(PS[2][0:NB4].pp(0, 1), rowB[127:128].pp(0, NB4), ident[0:NB4].pp(0, NB4))
    K.copy("dve", r0[0, 0:NB4].pp(0, 1), PS[2][0:NB4].pp(0, 1))
    K.tr(PS[1][0:128].pp(0, NB4), BC, ident)
    K.copy("dve", rowA.pp(0, NB4), PS[1][0:128].pp(0, NB4))
    K.tr(PS[2][0:NB4].pp(0, 1), rowA[127:128].pp(0, NB4), ident[0:NB4].pp(0, NB4))
    K.copy("dve", r0[1, 0:NB4].pp(0, 1), PS[2][0:NB4].pp(0, 1))

    def gate_prep():
        P0 = lambda v: v.pp(0, 1)
        for gb_ in range(NB):
            a, b = gb_ * 4, gb_ * 4 + 4
            K.tt("dve", P0(r0[2, b:b + 4]), P0(r0[2, a:b]), P0(r0[0, a:b]), ALU.max)
            K.tt("dve", P0(r0[2, b:b + 4]), P0(r0[2, b:b + 4]), P0(r0[1, a:b]), ALU.add)
        K.mm(PS[3][0:NB4 + 4], ones_f.pp(0, 1), P0(r0[2]))
        K.copy("dve", rep, PS[3][0:NB4])
        K.copy("dve", tmpc, PS[3][4:NB4 + 4])
        K.tr(PS[2][0:1].pp(0, NB4), P0(r0[2, 0:NB4]), ident[0:1].pp(0, 1))
        K.copy("dve", sm_[0:1].pp(0, NB4), PS[2][0:1].pp(0, NB4))
        K.ts("dve", rowA.pp(0, NB4), rowB.pp(0, NB4), sm_[0:1].pp(0, NB4), ALU.max)
        K.tr(PS[1][0:NB4], rowA.pp(0, NB4), ident[0:NB4].pp(0, NB4))
        K.copy("dve", MMt, PS[1][0:NB4])
        K.tt("dve", GS, rep, MMt, ALU.subtract)
        K.act(GS, GS, AF.Exp)
        K.ts("dve", GS, GS, DKS, ALU.mult)
        K.tt("dve", ENM, MMt, BC, ALU.add)
        K.act(ENM, ENM, AF.Exp, scale=-1.0)
        K.mm(PS[3][0:NB4], ones_f.pp(0, 1), P0(r0[1, 0:NB4]))
        K.tt("dve", GL, PS[3][0:NB4], tmpc, ALU.subtract)
        K.tt("dve", WL, Aa, GL, ALU.add)
        K.act(WL, WL, AF.Exp)
        K.tt("dve", GL, GL, rep, ALU.add)
        K.act(GL, GL, AF.Exp)

    def mlstm_pass(full):
        for gb_ in range(NB):
            i, blk = gb_ // 4, gb_ % 4
            sb = SBv(i)
            cols = slice(blk * 128, (blk + 1) * 128)
            if full and blk == 0:
                for c in range(4):
                    for kc in range(8):
                        K.mm(PS[0], wino[kc, c * 128:(c + 1) * 128], sb[kc], start=(kc == 0), stop=(kc == 7))
                    K.copy("act", QTl[c], PS[0])
                    for kc in range(8):
                        K.mm(PS[1], wino[kc, 512 + c * 128:512 + (c + 1) * 128], sb[kc], start=(kc == 0), stop=(kc == 7))
                    K.copy("dve", KTl[c], PS[1])
            for kc in range(8):
                K.mm(PS[0], sb[kc, cols], wino[kc, 512:1024], start=(kc == 0), stop=(kc == 7))
            K.copy("act", Ktm, PS[0])
            for hv in range(2):
                for kc in range(8):
                    K.mm(PS[1 + hv], sb[kc, cols], wino[kc, 1024 + hv * 512:1536 + hv * 512], start=(kc == 0), stop=(kc == 7))
                K.copy("act" if hv == 0 else "dve", Va.w(Va.ap[:, 2 * hv:2 * hv + 2, 0:256]),
                       PS[1 + hv].w(PS[1 + hv].ap.rearrange("p (h e) -> p h e", h=2)))
            if full:
                K.tr(PS[3][0:128].pp(0, 4), MMt[gb_ * 4:gb_ * 4 + 4], ident)
                K.copy("dve", mmr.pp(0, 4), PS[3][0:128].pp(0, 4))
            for h in range(4):
                ci = gb_ * 4 + h
                if full:
                    K.mm(PS[4][0:128], KTl[h, cols], QTl[h, cols])
                    K.mm(PS[5][0:128], sel4[h].pp(0, 4), mmr.pp(0, 4))
                    K.act(Et, PS[5][0:128], AF.Exp, scale=-1.0, bias=Aa[ci:ci + 1])
                    K.asel(Et, Et, [[1, 128]], ALU.is_ge, 0.0, 0, -1)
                    K.stt("dve", SWt, PS[4][0:128], DKS, Et, ALU.mult, ALU.mult)
                    K.mm(PS[6][0:257], SWt, Va[h])
                    K.mm(PS[7][0:257], QTl[h, cols], Cb[h])
                    K.copy("dve", sm_[1:2], PS[6][256:257])
                    K.stt("dve", sm_[2:3], PS[7][256:257], GS[ci:ci + 1], sm_[1:2], ALU.mult, ALU.add)
                    K.ts("dve", sm_[3:4], sm_[2:3], -1.0, ALU.mult)
                    K.tt("dve", sm_[3:4], sm_[3:4], sm_[2:3], ALU.max)
                    K.tt("dve", sm_[3:4], sm_[3:4], ENM[ci:ci + 1], ALU.max)
                    K.recip(sm_[4:5], sm_[3:4])
                    K.tt("dve", sm_[5:6], sm_[4:5], GS[ci:ci + 1], ALU.mult)
                    K.act(App, PS[7][0:256], AF.Copy, scale=sm_[5:6])
                    K.stt("dve", Hh[h * 256:(h + 1) * 256], PS[6][0:256], sm_[4:5], App, ALU.mult, ALU.add)
                K.ts("dve", Kw, Ktm[h * 128:(h + 1) * 128], WL[ci:ci + 1], ALU.mult)
                K.mm(PS[2][0:257], Kw, Va[h])
                K.stt("dve", Cf[h], Cf[h], GL[ci:ci + 1], PS[2][0:257], ALU.mult, ALU.add)
                K.copy("pool", Cb[h], Cf[h])
            if full:
                for hv in range(2):
                    for kc in range(8):
                        K.mm(PS[hv], sb[kc, cols], wino[kc, 2048 + hv * 512:2560 + hv * 512], start=(kc == 0), stop=(kc == 7))
                    K.act(sg1[hv * 512:(hv + 1) * 512], PS[hv], AF.Sigmoid)
                for h in range(4):
                    hs = Hh[h * 256:(h + 1) * 256]
                    S.add("dve", lambda e, o=bst[0:6].ap, a=hs.ap: e.bn_stats(out=o, in_=a), reads=[hs], writes=[bst[0:6]])
                    S.add("dve", lambda e, o=bst[6:8].ap, a=bst[0:6].ap: e.bn_aggr(out=o, in_=a), reads=[bst[0:6]], writes=[bst[6:8]])
                    K.act(sm_[6:7], bst[7:8], AF.Sqrt, bias=small[63:64])
                    K.recip(sm_[7:8], sm_[6:7])
                    K.ts("dve", hs, hs, bst[6:7], ALU.subtract, s2=sm_[7:8], op1=ALU.mult)
                K.tt("pool", Hh, Hh, mhw, ALU.mult)
                K.tt("pool", Hh, Hh, sg1, ALU.mult)
                for half in range(2):
                    for c in range(4):
                        fcn = half * 4 + c
                        K.tr(PS[3 + half][c * 128:(c + 1) * 128], Hh[fcn * 128:(fcn + 1) * 128], ident)
                for half in range(2):
                    K.copy("act" if half == 0 else "dve", sb.w(sb.ap[:, half * 4:(half + 1) * 4, cols]),
                           PS[3 + half].w(PS[3 + half].ap.rearrange("p (c t) -> p c t", c=4)))

    K.memset("dve", Cf, 0.0); K.memset("pool", Cb, 0.0); K.memset("dve", r0[2], 0.0)
    gate_prep()
    mlstm_pass(False)
    stg_ = SBA.view(Hh.off, [1032], F32)
    K.copy("dve", stg_[0:1028], Cf.w(Cf.ap.rearrange("p h e -> p (h e)")))
    K.memset("pool", stg_[1028:1032], 0.0)
    K.copy("dve", stg_[1028:1032].pp(0, 1), r0[2, NB4:NB4 + 4].pp(0, 1))
    K.dma("sp", V_stin, stg_)
    S.add("pool", lambda e: e.collective_compute("AllGather", ALU.bypass, replica_groups=[[0, 1], [2, 3], [4, 5], [6, 7]],
                                                 ins=[st_in.ap().opt()], outs=[st_out.ap().opt()]),
          reads=[V_stin], writes=[V_stout], dma=True, inc=1, semkey=101)
    K.dma("sp", stg_, V_stout.w(st_out.ap()[0:128, :]))
    K.ts("dve", Cf.w(Cf.ap.rearrange("p h e -> p (h e)")), stg_[0:1028], cfl[0:1], ALU.mult)
    K.copy("pool", Cb, Cf)
    K.memset("dve", r0[2], 0.0)
    K.ts("dve", r0[2, 0:4].pp(0, 1), stg_[1028:1032].pp(0, 1), cfl[0:1].pp(0, 1), ALU.mult)
    gate_prep()
    mlstm_pass(True)
    for h in range(4):
        K.dma("sp", U(o_Cp[h]), Cf[h, 0:256])
        K.dma("sp", U(o_np[h:h + 1, :].rearrange("o d -> d o")), Cf[h, 256:257])
    K.dma("sp", U(o_mp), r0[2, NB4:NB4 + 4].pp(0, 1))
    if stop <= 4:
        S.emit(nc, st); st.close(); return nc, ins
    sbs = SBv(NT)
    WK.cur = mhw.off + 4096
    sm_ = WK.alloc([32], F32); bst = WK.alloc([8], F32)
    qk_s = WK.alloc([1024], F32); sgs = WK.alloc([1024], F32); Hs = WK.alloc([1024], F32)
    Vsa = [WK.alloc([4, 257], BF16) for _ in range(2)]
    vs_f = WK.alloc([1024], F32)
    QsT = WK.alloc([4, 32], F32); KsT = WK.alloc([4, 32], F32); WKT = WK.alloc([4, 32], F32)
    gts = WK.alloc([64], F32)
    REP = WK.alloc([2, 4, 32], F32)
    Dm = WK.alloc([2, 4, 32], F32)
    i32r = WK.alloc([32, 32], F32); K.dma("sp", i32r, U(d_ident[0:32, 0:32].partition_broadcast(128)))
    Qd = WK.alloc([32, 32], F32)
    caug = [WK.alloc([257], F32) for _ in range(2)]
    ctmp = [WK.alloc([257], F32) for _ in range(2)]
    P32 = lambda v: v.pp(0, 32)
    for hv in range(2):
        for kc in range(8):
            K.mm(PS[hv].pp(0, 32), sbs[kc], wino[kc, hv * 512:(hv + 1) * 512], start=(kc == 0), stop=(kc == 7))
        K.copy("act", P32(qk_s[hv * 512:(hv + 1) * 512]), PS[hv].pp(0, 32))
    for hv in range(2):
        for kc in range(8):
            K.mm(PS[2 + hv].pp(0, 32), sbs[kc], wino[kc, 1024 + hv * 512:1536 + hv * 512], start=(kc == 0), stop=(kc == 7))
        K.copy("dve", P32(vs_f[hv * 512:(hv + 1) * 512]), PS[2 + hv].pp(0, 32))
    K.memset("pool", Vsa[0].w(Vsa[0].ap[:, :, 256:257]), 1.0)
    K.memset("pool", Vsa[1].w(Vsa[1].ap[:, :, 256:257]), 1.0)
    K.copy("act", Vsa[0].w(Vsa[0].ap[0:16, :, 0:256]), vs_f.w(vs_f.ap[0:16].rearrange("p (h e) -> p h e", h=4)))
    for hv in range(2):
        for kc in range(8):
            K.mm(PS[4 + hv].pp(0, 16), sbs[kc, 16:32], wino[kc, 1024 + hv * 512:1536 + hv * 512], start=(kc == 0), stop=(kc == 7))
        K.copy("act", Vsa[1].w(Vsa[1].ap[0:16, 2 * hv:2 * hv + 2, 0:256]),
               PS[4 + hv].w(PS[4 + hv].ap[0:16].rearrange("p (h e) -> p h e", h=2)))
    for hv in range(2):
        for kc in range(8):
            K.mm(PS[6 + hv].pp(0, 32), sbs[kc], wino[kc, 2048 + hv * 512:2560 + hv * 512], start=(kc == 0), stop=(kc == 7))
        K.act(P32(sgs[hv * 512:(hv + 1) * 512]), PS[6 + hv].pp(0, 32), AF.Sigmoid)
    for kc in range(8):
        K.mm(PS[0][0:8].pp(0, 32), sbs[kc], wino[kc, 3072:3080], start=(kc == 0), stop=(kc == 7))
    K.tt("dve", P32(gts[0:4]), PS[0][0:4].pp(0, 32), P32(bgr[0:4]), ALU.add)
    K.tt("dve", P32(gts[4:8]), PS[0][4:8].pp(0, 32), P32(bgr[4:8]), ALU.add)
    K.act(P32(gts[4:8]), P32(gts[4:8]), AF.Exp, scale=-1.0)
    K.act(P32(gts[4:8]), P32(gts[4:8]), AF.Ln, bias=1.0)
    K.ts("dve", P32(gts[4:8]), P32(gts[4:8]), -1.0, ALU.mult)
    K.dma("sp", P32(gts[8:12]), U(d_sm))
    K.tt("dve", P32(gts[12:16]), P32(gts[4:8]), P32(gts[8:12]), ALU.add)
    K.tt("dve", P32(gts[16:20]), P32(gts[12:16]), P32(gts[0:4]), ALU.max)
    K.dma("sp", U(o_ms), P32(gts[16:20]))
    K.tt("dve", P32(gts[20:24]), P32(gts[0:4]), P32(gts[16:20]), ALU.subtract)
    K.act(P32(gts[20:24]), P32(gts[20:24]), AF.Exp)
    K.tt("dve", P32(gts[24:28]), P32(gts[12:16]), P32(gts[16:20]), ALU.subtract)
    K.act(P32(gts[24:28]), P32(gts[24:28]), AF.Exp)
    K.act(P32(gts[28:32]), P32(gts[16:20]), AF.Exp, scale=-1.0)
    K.tt("dve", P32(Hs[0:512]), P32(qk_s[0:512]), P32(qk_s[512:1024]), ALU.mult)
    K.red("dve", P32(gts[32:36]), Hs.w(Hs.ap[0:32, 0:512].rearrange("p (h d) -> p h d", h=4)), ALU.add)
    K.tt("dve", P32(gts[36:40]), P32(gts[32:36]), P32(gts[20:24]), ALU.mult)
    K.ts("dve", P32(gts[36:40]), P32(gts[36:40]), DKS, ALU.mult)
    K.ts("dve", P32(gts[40:44]), P32(gts[24:28]), DKS, ALU.mult)
    for which, src in enumerate((gts[20:24], gts[24:28])):
        K.tt("dve", P32(Dm[which]), src.w(src.ap[0:32].unsqueeze(2).broadcast_to([32, 4, 32])),
             ident.w(ident.ap[0:32, 0:32].unsqueeze(1).broadcast_to([32, 4, 32])), ALU.mult)
        K.mm(PS[1][0:128], P32(ones_f), P32(Dm[which].w(Dm[which].ap.rearrange("p h j -> p (h j)"))))
        K.copy("dve", REP[which].w(REP[which].ap.rearrange("p h j -> p (h j)")), PS[1][0:128])
    for c in range(4):
        for kc in range(8):
            K.mm(PS[2][0:32], wino[kc, c * 128:(c + 1) * 128], sbs[kc], start=(kc == 0), stop=(kc == 7))
        K.copy("dve", QsT[c], PS[2][0:32])
        for kc in range(8):
            K.mm(PS[3][0:32], wino[kc, 512 + c * 128:512 + (c + 1) * 128], sbs[kc], start=(kc == 0), stop=(kc == 7))
        K.copy("dve", KsT[c], PS[3][0:32])
    K.tt("dve", WKT, KsT, REP[0], ALU.mult)
    for h in range(4):
        K.tt("dve", Qd, QsT[h].w(QsT[h].ap.unsqueeze(2).broadcast_to([128, 32, 32])), i32r, ALU.mult)
        for j in range(32):
            ca, ct = caug[j % 2], ctmp[j % 2]
            K.dma("sp", ca[0:256], U(d_sC[j, h]))
            K.dma("sp", ca[256:257], U(d_sn[j, h:h + 1, :].rearrange("o d -> d o")))
            K.mm(PS[4][0:257].pp(0, 32), Qd[j], ca, start=(j == 0), stop=(j == 31))
            K.mm(PS[5 + j % 2][0:257], sel16b[j % 16].pp(0, 16), Vsa[j // 16][h].pp(0, 16))
            K.ts("pool", ct, ca, REP[1, h, j:j + 1], ALU.mult)
            K.stt("dve", ct, PS[5 + j % 2][0:257], WKT[h, j:j + 1], ct, ALU.mult, ALU.add)
            K.dma("sp", U(o_Cs[j, h]), ct[0:256])
            K.dma("sp", U(o_ns[j, h:h + 1, :].rearrange("o d -> d o")), ct[256:257])
        K.copy("dve", P32(gts[44:45]), PS[4][256:257].pp(0, 32))
        K.stt("dve", P32(gts[45:46]), P32(gts[44:45]), P32(gts[40 + h:41 + h]), P32(gts[36 + h:37 + h]), ALU.mult, ALU.add)
        K.ts("dve", P32(gts[46:47]), P32(gts[45:46]), -1.0, ALU.mult)
        K.tt("dve", P32(gts[46:47]), P32(gts[46:47]), P32(gts[45:46]), ALU.max)
        K.tt("dve", P32(gts[46:47]), P32(gts[46:47]), P32(gts[28 + h:29 + h]), ALU.max)
        K.recip(P32(gts[47:48]), P32(gts[46:47]))
        hsl = P32(Hs[h * 256:(h + 1) * 256])
        K.ts("dve", hsl, P32(vs_f[h * 256:(h + 1) * 256]), P32(gts[36 + h:37 + h]), ALU.mult)
        K.stt("dve", hsl, PS[4][0:256].pp(0, 32), P32(gts[40 + h:41 + h]), hsl, ALU.mult, ALU.add)
        K.ts("dve", hsl, hsl, P32(gts[47:48]), ALU.mult)
        S.add("dve", lambda e, o=P32(bst[0:6]).ap, a=hsl.ap: e.bn_stats(out=o, in_=a), reads=[hsl], writes=[bst[0:6]])
        S.add("dve", lambda e, o=P32(bst[6:8]).ap, a=P32(bst[0:6]).ap: e.bn_aggr(out=o, in_=a), reads=[bst[0:6]], writes=[bst[6:8]])
        K.act(P32(sm_[6:7]), P32(bst[7:8]), AF.Sqrt, bias=P32(small[63:64]))
        K.recip(P32(sm_[7:8]), P32(sm_[6:7]))
        K.ts("dve", hsl, hsl, P32(bst[6:7]), ALU.subtract, s2=P32(sm_[7:8]), op1=ALU.mult)
    K.tt("pool", P32(Hs), P32(Hs), P32(mhw), ALU.mult)
    K.tt("pool", P32(Hs), P32(Hs), P32(sgs), ALU.mult)
    for half in range(2):
        for c in range(4):
            fcn = half * 4 + c
            K.tr(PS[half][c * 32:(c + 1) * 32], P32(Hs[fcn * 128:(fcn + 1) * 128]), ident[0:32].pp(0, 32))
    for half in range(2):
        K.copy("act" if half == 0 else "dve", sbs.w(sbs.ap[:, half * 4:(half + 1) * 4, :]),
               PS[half].w(PS[half].ap[:, 0:128].rearrange("p (c t) -> p c t", c=4)))
    tail(1, d_woo, SBv, None)
    WK.cur = W0
    yst = [WK.alloc([1024], F32) for _ in range(2)]
    for (i, w) in TILES:
        sa = SAv(i)
        for blk in range((w + 127) // 128):
            nr = min(128, w - blk * 128)
            ys = yst[blk % 2]
            for half in range(2):
                for c in range(4):
                    K.tr(PS[half][c * 128:(c + 1) * 128].pp(0, nr), sa[half * 4 + c, blk * 128: blk * 128 + nr], ident)
            for half in range(2):
                K.copy("act" if half == 0 else "dve", ys[half * 512:(half + 1) * 512].pp(0, nr), PS[half].pp(0, nr))
            dst = o_yp[i * 512 + blk * 128: i * 512 + blk * 128 + nr, :] if i < NT else o_ys
            K.dma("sp", U(dst), ys.pp(0, nr))
    S.emit(nc, st)
    st.close()
    return nc, ins


_ROPE_THETA = 500000.0


def _rope_table(pos):
    half = 8
    inv = (np.float32(_ROPE_THETA) ** (-np.arange(half, dtype=np.float32) / np.float32(half))).astype(np.float32)
    ang = pos.astype(np.float32)[:, None] * inv[None, :]
    return np.concatenate([np.cos(ang), np.sin(ang)], axis=1).astype(np.float32)


def _prep_inputs(inp):
    f = lambda a: np.ascontiguousarray(np.asarray(a), dtype=np.float32)
    xpm = f(inp["x_prompt"]); B, SEQ, D = xpm.shape
    T = SEQ // 2
    xsm = f(inp["x_sample"])[:, 0, :]
    ck = f(inp["cache_k"])[0]; cv = f(inp["cache_v"])[0]
    NPOOL = ck.shape[0]
    pt = np.asarray(inp["page_table"]).astype(np.int32)
    NPG = pt.shape[1]; NG = NPG // 8
    ppm = f(inp["p_prompt"]); psm = f(inp["p_sample"])[:, :, 0, :]
    w_in_e = f(inp["w_in_even"])[0]
    lnrows = []
    for l in range(2):
        for nm in ("ln_mix_g", "ln_mix_b", "ln_ffn_g", "ln_ffn_b"):
            lnrows.append(f(inp[nm])[l].reshape(8, 128))
    rows = np.concatenate(lnrows + [f(inp["conv_w"])[0].reshape(12, 128), f(inp["subln_w"])[0].reshape(1, 128)], axis=0)
    lams = np.stack([f(inp["lambda_q1"])[0], f(inp["lambda_k1"])[0], f(inp["lambda_q2"])[0], f(inp["lambda_k2"])[0]], axis=0)
    w_rt = np.concatenate([f(inp["w_group"]), f(inp["w_router"])], axis=-1)
    ident = np.eye(128, dtype=np.float32)
    sel16 = np.zeros((16, 16, 128), np.float32)
    for e in range(16):
        sel16[e, e, :] = 1.0
    sel4 = np.zeros((4, 4, 128), np.float32)
    for e in range(4):
        sel4[e, e, :] = 1.0
    e2 = np.zeros((2, 16, 16), np.float32)
    for i in range(16):
        e2[:, i, i] = 1.0
    tri = np.triu(np.ones((128, 128), np.float32))
    common = dict(
        xs=xsm, psm=np.ascontiguousarray(psm), sconv=f(inp["state_conv"])[0].reshape(32, 1024),
        sC=f(inp["state_mlstm_C"])[0], sn=f(inp["state_mlstm_n"])[0], sm=f(inp["state_mlstm_m"])[0],
        w_in_e=w_in_e, rows=rows, lams=lams, subrow=f(inp["subln_w"])[0].reshape(1, 128),
        w_out_e=f(inp["w_out_even"])[0], w_in_o=f(inp["w_in_odd"])[0], b_gates=f(inp["b_gates_odd"])[0].reshape(1, 8),
        mhw=f(inp["mh_norm_w"])[0].reshape(1, 1024), w_out_o=f(inp["w_out_odd"])[0], w_rt=np.ascontiguousarray(w_rt),
        w_eg=f(inp["w_exp_gate"]), w_eu=f(inp["w_exp_up"]), w_ed=f(inp["w_exp_down"]),
        w_pp=f(inp["w_ple_proj"]), w_pg=f(inp["w_ple_gate"]),
        ident=ident, rope_p=_rope_table(np.arange(T)), rope_s=_rope_table(np.full((32,), NPG * 128)),
        sel16=sel16.reshape(16, 2048), m01=np.array([[1.0, 0.0], [0.0, -1.0]], np.float32),
        e2=e2.reshape(2, 256), tri=tri, sel4=sel4.reshape(4, 512),
    )
    maps = []
    for c in range(8):
        b, hf, h, sg = c // 2, c % 2, c % 4, c // 4
        cfl = np.zeros((128, 4), np.float32)
        cfl[:, 0] = hf
        cfl[:, 1] = 0.0 if hf else -30000.0
        cfl[:, 2] = np.arange(128) % 16
        place = np.zeros((16, 32), np.float32)
        place[np.arange(16), 16 * sg + np.arange(16)] = 1.0
        hmask = np.zeros((32, 4), np.float32)
        hmask[:, h] = 1.0
        ptl = pt[16 * sg:16 * sg + 16].reshape(16, NG, 8).transpose(2, 0, 1).reshape(8, 16 * NG)
        m = dict(common)
        m.update(
            xo=np.ascontiguousarray(xpm[b, hf * T:(hf + 1) * T]), xp=np.ascontiguousarray(xpm[b, 0:T]),
            xs16=np.ascontiguousarray(xsm[16 * sg:16 * sg + 16]),
            pp=np.ascontiguousarray(ppm[:, b, hf * T:(hf + 1) * T, :]),
            poolk=np.ascontiguousarray(ck[:, :, 2 * h:2 * h + 2, :]).reshape(NPOOL, 128, 128),
            poolv=np.ascontiguousarray(cv[:, :, h, :]),
            ptab=np.ascontiguousarray(ptl).astype(np.int32),
            w_qkv_s=np.ascontiguousarray(np.concatenate(
                [w_in_e[:, 1536 + 128 * h:1664 + 128 * h], w_in_e[:, 2048 + 128 * h:2176 + 128 * h],
                 w_in_e[:, 2560 + 128 * h:2688 + 128 * h]], axis=1)),
            rope_o=_rope_table(hf * T + np.arange(T)), cflags=cfl, place=place, hmask=hmask,
        )
        maps.append(m)
    return maps, T, NPG, NPOOL


_CACHE = {}


def kernel(**inp):
    maps, T, NPG, NPOOL = _prep_inputs(inp)
    key = (T, NPG, NPOOL)
    if key not in _CACHE:
        _CACHE[key] = build(T=T, NPG=NPG, NPOOL=NPOOL)
    nc, ins = _CACHE[key]
    in_maps = [{k: m[k] for k in ins} for m in maps]
    res = run_bass_kernel_spmd(nc, in_maps, core_ids=list(range(8)))
    R = res.results
    SEQ = 2 * T
    y_p = np.zeros((4, SEQ, 1024), np.float32); k_p = np.zeros((1, 4, SEQ, 8, 64), np.float32)
    v_p = np.zeros((1, 4, SEQ, 4, 128), np.float32); conv_p = np.zeros((1, 4, 2, 512), np.float32)
    C_p = np.zeros((1, 4, 4, 128, 256), np.float32); n_p = np.zeros((1, 4, 4, 128), np.float32); m_p = np.zeros((1, 4, 4), np.float32)
    for c in range(8):
        b, hf = c // 2, c % 2
        sl = slice(hf * T, (hf + 1) * T)
        y_p[b, sl] = R[c]["y_p"]
        k_p[0, b, sl] = R[c]["k_p"].reshape(T, 8, 64)
        v_p[0, b, sl] = R[c]["v_p"].reshape(T, 4, 128)
        if hf == 1:
            conv_p[0, b] = R[c]["conv_p"]; C_p[0, b] = R[c]["C_p"]; n_p[0, b] = R[c]["n_p"]; m_p[0, b] = R[c]["m_p"][0]
    r0 = R[0]
    return (y_p, r0["y_s"].reshape(32, 1, 1024).copy(), k_p, v_p, conv_p, C_p, n_p, m_p,
            r0["k_s"].reshape(1, 32, 1, 8, 64).copy(), r0["v_s"].reshape(1, 32, 1, 4, 128).copy(),
            r0["conv_s"].reshape(1, 32, 2, 512).copy(), r0["C_s"].reshape(1, 32, 4, 128, 256).copy(),
            r0["n_s"].reshape(1, 32, 4, 128).copy(), r0["m_s"].reshape(1, 32, 4).copy())
```
